# Optimizing a Trainium2 kernel written in Bass

```python
import math
import jax, jax.numpy as jnp
from jax import lax
import numpy as np

D_MODEL = 1024
BATCH = 8
SEQ = 4096
DEPTH = 1

S5_WIDTH = 512
S5_GROUP = 16
S5_GROUPS = S5_WIDTH // S5_GROUP
S5_STATE = 64
DT_MIN = 1e-3
DT_MAX = 1e-1
N_HEADS = 8
N_KV_HEADS = 2
GQA_GROUP = N_HEADS // N_KV_HEADS
HEAD_DIM = 64
NSA_WIDTH = N_HEADS * HEAD_DIM
KV_WIDTH = 2 * N_KV_HEADS * HEAD_DIM
CMP_BLOCK = 32
CMP_STRIDE = 16
CMP_HIDDEN = 256
SEL_BLOCK = 64
SEL_TOPK = 8
WINDOW = 256
Q_BLOCK = 128
FORCE_BONUS = 1e3
N_EXPERTS = 64
TOP_K = 8
N_EXPERT_GROUPS = 8
TOPK_EXPERT_GROUPS = 4
EXPERT_HIDDEN = 256
SHARED_HIDDEN = 256
ROUTED_SCALE = 2.5
MOE_BLOCK = 128
RMS_EPS = 1e-6
NEG_INF = -1e30
N_MOD = 6
IN_SPLITS = (S5_WIDTH, NSA_WIDTH, KV_WIDTH, KV_WIDTH, KV_WIDTH, 3 * N_HEADS, 2 * D_MODEL)
IN_WIDTH = sum(IN_SPLITS)

kernel_name = "hybrid_s5_nsa_moe_adaln_block"


def rms_norm(x, gain):
    xf = x.astype(jnp.float32)
    y = xf * lax.rsqrt(jnp.mean(xf * xf, axis=-1, keepdims=True) + RMS_EPS)
    return (y * gain.astype(jnp.float32)).astype(x.dtype)


def masked_softmax(s, mask):
    s = jnp.where(mask, s, NEG_INF)
    m = jnp.max(s, axis=-1, keepdims=True)
    p = jnp.where(mask, jnp.exp(s - m), 0.0)
    return p / jnp.maximum(jnp.sum(p, axis=-1, keepdims=True), 1e-20)


def alibi_slopes(n_heads):
    return jnp.asarray(2.0 ** (-8.0 * np.arange(1, n_heads + 1) / n_heads), jnp.float32)


def s5_mixer(u, lam_re, lam_im, log_dt, b_re, b_im, c_re, c_im, d_skip, w_glu, b_glu):
    f32 = jnp.float32
    Bsz, S, _ = u.shape
    uf = u.astype(f32).reshape(Bsz, S, S5_GROUPS, S5_GROUP)
    lr, li = lam_re.astype(f32), lam_im.astype(f32)
    dt = jnp.exp(log_dt.astype(f32))[:, None]
    mag = jnp.exp(lr * dt)
    abar_re, abar_im = mag * jnp.cos(li * dt), mag * jnp.sin(li * dt)
    num_re, num_im = abar_re - 1.0, abar_im
    den = lr * lr + li * li
    coef_re = (num_re * lr + num_im * li) / den
    coef_im = (num_im * lr - num_re * li) / den
    br, bi = b_re.astype(f32), b_im.astype(f32)
    bbar_re = coef_re[..., None] * br - coef_im[..., None] * bi
    bbar_im = coef_re[..., None] * bi + coef_im[..., None] * br
    bu_re = jnp.einsum('bsgp,gnp->bsgn', uf, bbar_re)
    bu_im = jnp.einsum('bsgp,gnp->bsgn', uf, bbar_im)
    a_re = jnp.broadcast_to(abar_re[None, None], (1, S, S5_GROUPS, S5_STATE))
    a_im = jnp.broadcast_to(abar_im[None, None], (1, S, S5_GROUPS, S5_STATE))

    def combine(e1, e2):
        a1r, a1i, b1r, b1i = e1
        a2r, a2i, b2r, b2i = e2
        return (a2r * a1r - a2i * a1i,
                a2r * a1i + a2i * a1r,
                a2r * b1r - a2i * b1i + b2r,
                a2r * b1i + a2i * b1r + b2i)

    _, _, x_re, x_im = lax.associative_scan(combine, (a_re, a_im, bu_re, bu_im), axis=1)
    y = (jnp.einsum('bsgn,gpn->bsgp', x_re, c_re.astype(f32))
         - jnp.einsum('bsgn,gpn->bsgp', x_im, c_im.astype(f32))
         + d_skip.astype(f32) * uf).reshape(Bsz, S, S5_WIDTH)
    z = jax.nn.gelu(y)
    out = z * jax.nn.sigmoid(z @ w_glu.astype(f32) + b_glu.astype(f32))
    return out.astype(u.dtype)


def nsa_mixer(q, kv_c, kv_s, kv_w, g_nsa, q_gain, k_gain, cmp_pe, cmp_w1, cmp_b1, cmp_w2, cmp_b2):
    f32 = jnp.float32
    Bsz, S, _ = q.shape
    dt = q.dtype
    scale = HEAD_DIM ** -0.5
    q = rms_norm(q.reshape(Bsz, S, N_HEADS, HEAD_DIM), q_gain).reshape(
        Bsz, S, N_KV_HEADS, GQA_GROUP, HEAD_DIM)

    def split_kv(kv):
        kv = kv.reshape(Bsz, S, 2, N_KV_HEADS, HEAD_DIM)
        return kv[:, :, 0], kv[:, :, 1]

    k_c, v_c = split_kv(kv_c)
    k_s, v_s = split_kv(kv_s)
    k_w, v_w = split_kv(kv_w)
    k_s = rms_norm(k_s, k_gain[1])
    k_w = rms_norm(k_w, k_gain[2])

    n_cmp = (S - CMP_BLOCK) // CMP_STRIDE + 1
    cmp_start = np.arange(n_cmp) * CMP_STRIDE
    cmp_idx = cmp_start[:, None] + np.arange(CMP_BLOCK)[None, :]
    cmp_pos = jnp.asarray(cmp_idx[:, -1], jnp.int32)

    def compress(t, j):
        blocks = t[:, cmp_idx] + cmp_pe[j][None, None, :, None, :]
        flat = blocks.transpose(0, 1, 3, 2, 4).reshape(Bsz, n_cmp, N_KV_HEADS, CMP_BLOCK * HEAD_DIM)
        hid = jax.nn.gelu(flat @ cmp_w1[j] + cmp_b1[j])
        return hid @ cmp_w2[j] + cmp_b2[j]

    kc = rms_norm(compress(k_c, 0), k_gain[0])
    vc = compress(v_c, 1)

    n_sel = S // SEL_BLOCK
    n_pick = min(SEL_TOPK, n_sel)
    ks_blocks = k_s.reshape(Bsz, n_sel, SEL_BLOCK, N_KV_HEADS, HEAD_DIM).transpose(0, 3, 1, 2, 4)
    vs_blocks = v_s.reshape(Bsz, n_sel, SEL_BLOCK, N_KV_HEADS, HEAD_DIM).transpose(0, 3, 1, 2, 4)
    sel_start = np.arange(n_sel) * SEL_BLOCK
    overlap = jnp.asarray(((cmp_start[:, None] <= sel_start[None, :] + SEL_BLOCK - 1)
                           & (cmp_idx[:, -1][:, None] >= sel_start[None, :])).astype(np.float32))

    pad = ((0, 0), (WINDOW, 0), (0, 0), (0, 0))
    kw_pad, vw_pad = jnp.pad(k_w, pad), jnp.pad(v_w, pad)

    slopes = alibi_slopes(N_HEADS).reshape(N_KV_HEADS, GQA_GROUP)
    gates = jax.nn.sigmoid(g_nsa.astype(f32)).astype(dt).reshape(Bsz, S, 3, N_KV_HEADS, GQA_GROUP)
    gather = jax.vmap(jax.vmap(lambda blk, ix: blk[ix]))

    def attend_block(qb):
        t0 = qb * Q_BLOCK
        qq = lax.dynamic_slice_in_dim(q, t0, Q_BLOCK, axis=1)
        gg = lax.dynamic_slice_in_dim(gates, t0, Q_BLOCK, axis=1)
        t = t0 + jnp.arange(Q_BLOCK, dtype=jnp.int32)
        dist_c = (t[:, None] - cmp_pos[None, :]).astype(f32)
        s_c = (jnp.einsum('bqhgd,bchd->bhgqc', qq, kc).astype(f32) * scale
               - slopes[:, :, None, None] * dist_c)
        p_c = masked_softmax(s_c, dist_c >= 0)
        o_c = jnp.einsum('bhgqc,bchd->bqhgd', p_c.astype(dt), vc)
        imp = jnp.einsum('bhgqc,cj->bhqj', p_c, overlap)
        cur = t // SEL_BLOCK
        jb = jnp.arange(n_sel)
        forced = (jb[None, :] == 0) | (jb[None, :] == cur[:, None]) | (jb[None, :] == cur[:, None] - 1)
        imp = jnp.where(forced, imp + FORCE_BONUS, imp)
        imp = jnp.where(jb[None, :] <= cur[:, None], imp, -1.0)
        _, sel = lax.top_k(imp, n_pick)
        ksel = gather(ks_blocks, sel).reshape(Bsz, N_KV_HEADS, Q_BLOCK, n_pick * SEL_BLOCK, HEAD_DIM)
        vsel = gather(vs_blocks, sel).reshape(Bsz, N_KV_HEADS, Q_BLOCK, n_pick * SEL_BLOCK, HEAD_DIM)
        pos_s = (sel[..., None] * SEL_BLOCK + jnp.arange(SEL_BLOCK)).reshape(
            Bsz, N_KV_HEADS, Q_BLOCK, n_pick * SEL_BLOCK)
        dist_s = (t[None, None, :, None] - pos_s)[:, :, None]
        s_s = (jnp.einsum('bqhgd,bhqkd->bhgqk', qq, ksel).astype(f32) * scale
               - slopes[None, :, :, None, None] * dist_s.astype(f32))
        p_s = masked_softmax(s_s, dist_s >= 0)
        o_s = jnp.einsum('bhgqk,bhqkd->bqhgd', p_s.astype(dt), vsel)
        kw = lax.dynamic_slice_in_dim(kw_pad, t0, WINDOW + Q_BLOCK, axis=1)
        vw = lax.dynamic_slice_in_dim(vw_pad, t0, WINDOW + Q_BLOCK, axis=1)
        pos_w = t0 - WINDOW + jnp.arange(WINDOW + Q_BLOCK, dtype=jnp.int32)
        dist_w = t[:, None] - pos_w[None, :]
        mask_w = (dist_w >= 0) & (dist_w < WINDOW) & (pos_w[None, :] >= 0)
        s_w = (jnp.einsum('bqhgd,bkhd->bhgqk', qq, kw).astype(f32) * scale
               - slopes[:, :, None, None] * dist_w.astype(f32))
        p_w = masked_softmax(s_w, mask_w)
        o_w = jnp.einsum('bhgqk,bkhd->bqhgd', p_w.astype(dt), vw)
        o = (gg[:, :, 0, :, :, None] * o_c + gg[:, :, 1, :, :, None] * o_s
             + gg[:, :, 2, :, :, None] * o_w)
        return o.reshape(Bsz, Q_BLOCK, NSA_WIDTH)

    out = lax.map(attend_block, jnp.arange(S // Q_BLOCK))
    return out.transpose(1, 0, 2, 3).reshape(Bsz, S, NSA_WIDTH)


def moe_ffn(h, w_router, router_bias, w_gate, w_up, w_down, ws_gate, ws_up, ws_down):
    Bsz, S, D = h.shape
    T = Bsz * S
    xt = h.reshape(T, D)
    scores = jax.nn.sigmoid((xt @ w_router).astype(jnp.float32))
    sel = scores + router_bias.astype(jnp.float32)
    grp = sel.reshape(T, N_EXPERT_GROUPS, N_EXPERTS // N_EXPERT_GROUPS)
    grp_score = lax.top_k(grp, 2)[0].sum(-1)
    _, top_groups = lax.top_k(grp_score, TOPK_EXPERT_GROUPS)
    group_mask = jnp.any(top_groups[..., None] == jnp.arange(N_EXPERT_GROUPS), axis=-2)
    expert_mask = jnp.repeat(group_mask, N_EXPERTS // N_EXPERT_GROUPS, axis=-1)
    _, top_e = lax.top_k(jnp.where(expert_mask, sel, NEG_INF), TOP_K)
    w = jnp.take_along_axis(scores, top_e, axis=-1)
    w = w / jnp.sum(w, axis=-1, keepdims=True) * ROUTED_SCALE

    A = T * TOP_K
    flat_e = top_e.reshape(A)
    flat_tok = jnp.repeat(jnp.arange(T, dtype=jnp.int32), TOP_K)
    flat_w = w.reshape(A)
    order = jnp.argsort(flat_e)
    e_sorted, tok_sorted, w_sorted = flat_e[order], flat_tok[order], flat_w[order]
    counts = jnp.bincount(flat_e, length=N_EXPERTS)
    padded = (counts + MOE_BLOCK - 1) // MOE_BLOCK * MOE_BLOCK
    pad_end = jnp.cumsum(padded)
    pad_start = pad_end - padded
    start = jnp.cumsum(counts) - counts
    dest = pad_start[e_sorted] + (jnp.arange(A) - start[e_sorted])
    P = A + N_EXPERTS * MOE_BLOCK
    n_blocks = P // MOE_BLOCK
    buf_tok = jnp.zeros((P,), jnp.int32).at[dest].set(tok_sorted)
    buf_w = jnp.zeros((P,), jnp.float32).at[dest].set(w_sorted)
    block_expert = jnp.minimum(
        jnp.searchsorted(pad_end, jnp.arange(n_blocks) * MOE_BLOCK, side='right'), N_EXPERTS - 1)

    def expert_block(args):
        tok, e, wt = args
        rows = xt[tok]
        hid = jax.nn.silu(rows @ w_gate[e]) * (rows @ w_up[e])
        return (hid @ w_down[e]) * wt[:, None].astype(rows.dtype)

    out = lax.map(expert_block, (buf_tok.reshape(n_blocks, MOE_BLOCK), block_expert,
                                 buf_w.reshape(n_blocks, MOE_BLOCK)))
    routed = jnp.zeros((T, D), xt.dtype).at[buf_tok].add(out.reshape(P, D))
    shared = (jax.nn.silu(xt @ ws_gate) * (xt @ ws_up)) @ ws_down
    return (routed + shared).reshape(Bsz, S, D)


def setup_inputs(seed: int = 0) -> dict:
    key = jax.random.key(seed)
    ks = iter(jax.random.split(key, 40))

    def nrm(shape, scale):
        return jax.random.normal(next(ks), shape, jnp.float32) * scale

    L, D = DEPTH, D_MODEL
    G, N, Pc = S5_GROUPS, S5_STATE, S5_GROUP
    n_idx = jnp.arange(N, dtype=jnp.float32)
    return {
        "x": nrm((BATCH, SEQ, D), 1.0),
        "c": nrm((BATCH, D), 1.0),
        "w_ada": nrm((L, D, N_MOD * D), 0.5 * D ** -0.5),
        "b_ada": nrm((L, N_MOD * D), 0.01),
        "norm_mix_gain": 1.0 + nrm((L, D), 0.01),
        "norm_ffn_gain": 1.0 + nrm((L, D), 0.01),
        "w_in": nrm((L, D, IN_WIDTH), D ** -0.5),
        "s5_lambda_re": -0.5 * (1.0 + nrm((L, G, N), 0.01)),
        "s5_lambda_im": math.pi * n_idx + nrm((L, G, N), 0.01),
        "s5_log_dt": jax.random.uniform(next(ks), (L, G), jnp.float32,
                                        math.log(DT_MIN), math.log(DT_MAX)),
        "s5_b_re": nrm((L, G, N, Pc), Pc ** -0.5),
        "s5_b_im": nrm((L, G, N, Pc), Pc ** -0.5),
        "s5_c_re": nrm((L, G, Pc, N), N ** -0.5),
        "s5_c_im": nrm((L, G, Pc, N), N ** -0.5),
        "s5_d": nrm((L, G, Pc), 0.5),
        "s5_w_glu": nrm((L, S5_WIDTH, S5_WIDTH), S5_WIDTH ** -0.5),
        "s5_b_glu": nrm((L, S5_WIDTH), 0.01),
        "q_norm_gain": 1.0 + nrm((L, HEAD_DIM), 0.01),
        "k_norm_gain": 1.0 + nrm((L, 3, HEAD_DIM), 0.01),
        "cmp_pe": nrm((L, 2, CMP_BLOCK, HEAD_DIM), 0.1),
        "cmp_w1": nrm((L, 2, CMP_BLOCK * HEAD_DIM, CMP_HIDDEN), (CMP_BLOCK * HEAD_DIM) ** -0.5),
        "cmp_b1": nrm((L, 2, CMP_HIDDEN), 0.01),
        "cmp_w2": nrm((L, 2, CMP_HIDDEN, HEAD_DIM), CMP_HIDDEN ** -0.5),
        "cmp_b2": nrm((L, 2, HEAD_DIM), 0.01),
        "w_branch_a": nrm((L, S5_WIDTH, D), S5_WIDTH ** -0.5),
        "w_branch_b": nrm((L, NSA_WIDTH, D), NSA_WIDTH ** -0.5),
        "w_out": nrm((L, D, D), D ** -0.5),
        "w_router": nrm((L, D, N_EXPERTS), D ** -0.5),
        "router_bias": nrm((L, N_EXPERTS), 0.01),
        "w_gate": nrm((L, N_EXPERTS, D, EXPERT_HIDDEN), D ** -0.5),
        "w_up": nrm((L, N_EXPERTS, D, EXPERT_HIDDEN), D ** -0.5),
        "w_down": nrm((L, N_EXPERTS, EXPERT_HIDDEN, D), EXPERT_HIDDEN ** -0.5),
        "ws_gate": nrm((L, D, SHARED_HIDDEN), D ** -0.5),
        "ws_up": nrm((L, D, SHARED_HIDDEN), D ** -0.5),
        "ws_down": nrm((L, SHARED_HIDDEN, D), SHARED_HIDDEN ** -0.5),
    }


def reference(x, c, w_ada, b_ada, norm_mix_gain, norm_ffn_gain, w_in,
              s5_lambda_re, s5_lambda_im, s5_log_dt, s5_b_re, s5_b_im, s5_c_re, s5_c_im,
              s5_d, s5_w_glu, s5_b_glu, q_norm_gain, k_norm_gain,
              cmp_pe, cmp_w1, cmp_b1, cmp_w2, cmp_b2,
              w_branch_a, w_branch_b, w_out, w_router, router_bias,
              w_gate, w_up, w_down, ws_gate, ws_up, ws_down):
    split_points = list(np.cumsum(IN_SPLITS)[:-1])
    for l in range(DEPTH):
        mod = jax.nn.silu(c) @ w_ada[l] + b_ada[l]
        shift_m, scale_m, gate_m, shift_f, scale_f, gate_f = jnp.split(mod[:, None, :], N_MOD, axis=-1)
        h = rms_norm(x, norm_mix_gain[l]) * (1.0 + scale_m) + shift_m
        u_s5, q, kv_c, kv_s, kv_w, g_nsa, g_merge = jnp.split(h @ w_in[l], split_points, axis=-1)
        y_a = s5_mixer(u_s5, s5_lambda_re[l], s5_lambda_im[l], s5_log_dt[l], s5_b_re[l], s5_b_im[l],
                       s5_c_re[l], s5_c_im[l], s5_d[l], s5_w_glu[l], s5_b_glu[l]) @ w_branch_a[l]
        y_b = nsa_mixer(q, kv_c, kv_s, kv_w, g_nsa, q_norm_gain[l], k_norm_gain[l],
                        cmp_pe[l], cmp_w1[l], cmp_b1[l], cmp_w2[l], cmp_b2[l]) @ w_branch_b[l]
        g_a, g_b = jnp.split(jax.nn.sigmoid(g_merge), 2, axis=-1)
        x = x + gate_m * ((g_a * y_a + g_b * y_b) @ w_out[l])
        h = rms_norm(x, norm_ffn_gain[l]) * (1.0 + scale_f) + shift_f
        x = x + gate_f * moe_ffn(h, w_router[l], router_bias[l], w_gate[l], w_up[l], w_down[l],
                                 ws_gate[l], ws_up[l], ws_down[l])
    return x
```

```python
import numpy as np
from contextlib import ExitStack
import concourse.bass as bass
import concourse.mybir as mybir
from concourse.bass_utils import run_bass_kernel_spmd

F32 = mybir.dt.float32
BF16 = mybir.dt.bfloat16
AF = mybir.ActivationFunctionType
ALU = mybir.AluOpType
AX = mybir.AxisListType

S = 4096
D = 1024
NT = S // 128
INW = 3864
RMS_EPS = 1e-6
ENGS = ("pe", "act", "dve", "pool", "sp")
N_DSEM = 24
import os as _os
NOSAME = bool(_os.environ.get('NOSAME'))


class Sched:
    def __init__(self):
        self.ops = {e: [] for e in ENGS}
        self.cnt = {e: 0 for e in ENGS}
        self.waited = {e: {} for e in ENGS}
        self.res = {}
        self.dcnt = [0] * N_DSEM
        self.dnext = 0

    def _collect(self, reads, writes):
        evs = {}

        def add(k, v):
            if v > evs.get(k, 0):
                evs[k] = v
        for r in reads:
            st = self.res.get(r)
            if st and st["w"]:
                add(*st["w"])
        for w in writes:
            st = self.res.get(w)
            if st:
                if st["w"]:
                    add(*st["w"])
                for k, v in st["r"].items():
                    add(k, v)
        return evs

    def _commit(self, ev, reads, writes):
        for r in reads:
            st = self.res.setdefault(r, {"w": None, "r": {}})
            if ev[1] > st["r"].get(ev[0], 0):
                st["r"][ev[0]] = ev[1]
        for w in writes:
            self.res[w] = {"w": ev, "r": {}}

    def _waits(self, eng, evs):
        out = []
        for k, v in evs.items():
            if isinstance(k, int):
                v = self.dcnt[k]
            elif k == eng and (eng == "pe" or NOSAME):
                continue
            if self.waited[eng].get(k, 0) >= v:
                continue
            self.waited[eng][k] = v
            out.append((k, v))
        return out

    def op(self, eng, fn, reads=(), writes=()):
        waits = self._waits(eng, self._collect(reads, writes))
        self.cnt[eng] += 1
        ev = (eng, self.cnt[eng])
        self.ops[eng].append((waits, fn, (eng, 1)))
        self._commit(ev, reads, writes)

    def dma(self, fn, reads=(), writes=(), q="sp"):
        waits = self._waits(q, self._collect(reads, writes))
        s = self.dnext
        self.dnext = (self.dnext + 1) % N_DSEM
        self.dcnt[s] += 16
        ev = (s, self.dcnt[s])
        self.ops[q].append((waits, fn, (s, 16)))
        self._commit(ev, reads, writes)

    def barrier(self):
        for e in ENGS:
            waits = []
            for k in ENGS:
                v = self.cnt[k]
                if v > self.waited[e].get(k, 0):
                    self.waited[e][k] = v
                    waits.append((k, v))
            for s in range(N_DSEM):
                v = self.dcnt[s]
                if v > self.waited[e].get(s, 0):
                    self.waited[e][s] = v
                    waits.append((s, v))
            if waits:
                self.ops[e].append((waits, None, None))
        self.res = {}

    def emit(self, nc, sems, dsems):
        def run(engname, eng):
            for waits, fn, inc in self.ops[engname]:
                for k, v in waits:
                    eng.wait_ge(dsems[k] if isinstance(k, int) else sems[k], v)
                if fn is None:
                    continue
                ins = fn(eng)
                k, n = inc
                ins.then_inc(dsems[k] if isinstance(k, int) else sems[k], n)
        return run


ALL_PHASES = ("0", "ab", "s5", "nsa", "e", "moe")


class Ctx:
    pass


_UNIQ = [0]


def uniq(name):
    _UNIQ[0] += 1
    return "%s_u%d" % (name, _UNIQ[0])


def mk_helpers(sc):
    H = Ctx()

    def tt(o, a, b, op, r, w, eng="dve"):
        sc.op(eng, lambda e: e.tensor_tensor(out=o, in0=a, in1=b, op=op), r, w)

    def ts(o, a, s1, s2, op0, op1, r, w, eng="dve"):
        if op1 is None:
            sc.op(eng, lambda e: e.tensor_scalar(out=o, in0=a, scalar1=s1, scalar2=None, op0=op0), r, w)
        else:
            sc.op(eng, lambda e: e.tensor_scalar(out=o, in0=a, scalar1=s1, scalar2=s2, op0=op0, op1=op1), r, w)

    def stt(o, a, s, b, op0, op1, r, w):
        sc.op("dve", lambda e: e.scalar_tensor_tensor(out=o, in0=a, scalar=s, in1=b, op0=op0, op1=op1), r, w)

    def act(o, a, func, r, w, **kw):
        sc.op("act", lambda e: e.activation(out=o, in_=a, func=func, **kw), r, w)

    def cp(o, a, r, w, eng="dve"):
        if eng == "act":
            sc.op("act", lambda e: e.activation(out=o, in_=a, func=AF.Identity), r, w)
        else:
            sc.op(eng, lambda e: e.tensor_copy(out=o, in_=a), r, w)

    def ms(o, v, w, eng="dve"):
        sc.op(eng, lambda e: e.memset(o, v), [], w)

    def mm(o, l, rr, st, sp, r, w):
        sc.op("pe", lambda e: e.matmul(o, lhsT=l, rhs=rr, start=st, stop=sp), r, w)

    def tr(o, a, ident, r, w):
        sc.op("pe", lambda e: e.transpose(out=o, in_=a, identity=ident), r, w)

    def dma(o, a, r, w, q="sp"):
        sc.dma(lambda e: e.dma_start(out=o, in_=a), reads=r, writes=w, q=q)
    H.tt, H.ts, H.stt, H.act, H.cp, H.ms, H.mm, H.tr, H.dma = tt, ts, stt, act, cp, ms, mm, tr, dma
    return H


def phase_s5(C):
    nc, sc = C.nc, C.sc
    H = mk_helpers(sc)
    tt, ts, stt, act, cp, ms, mm, tr, dma = H.tt, H.ts, H.stt, H.act, H.cp, H.ms, H.mm, H.tr, H.dma
    M, A, SUB = ALU.mult, ALU.add, ALU.subtract
    P = ["prm"]
    lamr_d = C.din("s5_lamr", [128, 16]); lami_d = C.din("s5_lami", [128, 16]); ldt_d = C.din("s5_ldt", [128, 16])
    bre_d = C.din("s5_bre", [128, 256]); bim_d = C.din("s5_bim", [128, 256])
    cre_d = C.din("s5_cre", [128, 256]); cim_d = C.din("s5_cim", [128, 256])
    dcol_d = C.din("s5_dcol", [128, 32]); mask_d = C.din("mask01", [128, 4])
    wglu_d = C.din("s5_wglu", [512, 512]); bgl_d = C.din("s5_bgl", [128, 4])
    with ExitStack() as ph:
        sbp = lambda name, shape, dt=F32: ph.enter_context(nc.sbuf_tensor(uniq(name), list(shape), dt))
        BsT = sbp("BsT", [128, 32, 2, 128], BF16)
        EEm = sbp("EEm", [128, 32, 2, 144], BF16)
        Tb = sbp("Tb", [128, 32, 128], BF16)
        CPr = sbp("CPr", [128, 16, 9]); CPi = sbp("CPi", [128, 16, 9]); CPin = sbp("CPin", [128, 16, 9])
        wglu = sbp("wglu", [128, 4, 512], BF16); bgl = sbp("bgl", [128, 4])
        with ExitStack() as pr:
            sbr = lambda name, shape, dt=F32: pr.enter_context(nc.sbuf_tensor(uniq(name), list(shape), dt))
            psr = lambda name, shape, dt=F32: pr.enter_context(nc.psum_tensor(uniq(name), list(shape), dt))
            T_ = {n: sbr("q_" + n, [128, 16]) for n in
                  "lamr lami ldt dt magl ang m16 s16 c16 pr pi t1 t2 t3 numr den rden cr ci".split()}
            halfpi = sbr("halfpi", [128, 1])
            mask = sbr("mask", [128, 4])
            dcol = sbr("dcol", [128, 32])
            Bre = sbr("Bre", [128, 16, 16]); Bim = sbr("Bim", [128, 16, 16])
            Cre = sbr("Cre", [128, 16, 16]); Cim = sbr("Cim", [128, 16, 16])
            bbr = sbr("bbr", [128, 16, 16]); bbi = sbr("bbi", [128, 16, 16])
            bbrb = sbr("bbrb", [128, 16, 16], BF16); bbib = sbr("bbib", [128, 16, 16], BF16)
            tA = sbr("tA", [128, 16, 16]); tB = sbr("tB", [128, 16, 16])
            PWr = sbr("PWr", [128, 16, 9]); PWi = sbr("PWi", [128, 16, 9])
            Wr = sbr("Wr", [128, 16, 128]); Wi = sbr("Wi", [128, 16, 128])
            EEr = sbr("EEr", [128, 16, 144]); EEi = sbr("EEi", [128, 16, 144])
            Kall = sbr("Kall", [16, 32, 128])
            T32 = sbr("T32", [128, 32, 128])
            wgst = sbr("wgst", [128, 4, 512])
            ptf = psr("ptf", [128, 4, 128])
            pk = psr("pk", [16, 4, 128])
            for nm, dd in (("lamr", lamr_d), ("lami", lami_d), ("ldt", ldt_d)):
                dma(T_[nm][:], dd, [], P)
            dma(Bre[:].rearrange("p a b -> p (a b)"), bre_d, [], P); dma(Bim[:].rearrange("p a b -> p (a b)"), bim_d, [], P)
            dma(Cre[:].rearrange("p a b -> p (a b)"), cre_d, [], P); dma(Cim[:].rearrange("p a b -> p (a b)"), cim_d, [], P)
            dma(dcol[:], dcol_d, [], P); dma(mask[:], mask_d, [], P); dma(bgl[:], bgl_d, [], ["bgl"])
            dma(wgst[:], wglu_d.rearrange("(kc p) n -> p kc n", p=128), [], ["wgst"])
            cp(wglu[:], wgst[:], ["wgst"], ["wglu"], eng="pool")
            ms(halfpi[:], float(np.pi / 2), P)
            g = lambda n: T_[n][:]
            act(g("dt"), g("ldt"), AF.Exp, P, P)
            tt(g("magl"), g("lamr"), g("dt"), M, P, P); tt(g("ang"), g("lami"), g("dt"), M, P, P)
            act(g("m16"), g("magl"), AF.Exp, P, P, scale=1.0 / 16)
            act(g("s16"), g("ang"), AF.Sin, P, P, scale=1.0 / 16)
            act(g("c16"), g("ang"), AF.Sin, P, P, scale=1.0 / 16, bias=halfpi[:])
            tt(g("pr"), g("m16"), g("c16"), M, P, P); tt(g("pi"), g("m16"), g("s16"), M, P, P)

            def csq(r, i):
                tt(g("t1"), r, r, M, P, P); tt(g("t2"), i, i, M, P, P)
                stt(g("t3"), r, 2.0, i, M, M, P, P)
                tt(r, g("t1"), g("t2"), SUB, P, P); cp(i, g("t3"), P, P)
            for _ in range(4):
                csq(g("pr"), g("pi"))
            ms(PWr[:, :, 0:1], 1.0, P); ms(PWi[:, :, 0:1], 0.0, P)
            cp(PWr[:, :, 1], g("pr"), P, P); cp(PWi[:, :, 1], g("pi"), P, P)
            for k in range(2, 9):
                tt(g("t1"), PWr[:, :, k - 1], g("pr"), M, P, P); tt(g("t2"), PWi[:, :, k - 1], g("pi"), M, P, P)
                tt(PWr[:, :, k], g("t1"), g("t2"), SUB, P, P)
                tt(g("t1"), PWr[:, :, k - 1], g("pi"), M, P, P); tt(g("t2"), PWi[:, :, k - 1], g("pr"), M, P, P)
                tt(PWi[:, :, k], g("t1"), g("t2"), A, P, P)
            cp(CPr[:, :, 0], PWr[:, :, 8], P, P); cp(CPi[:, :, 0], PWi[:, :, 8], P, P)
            for s_ in range(1, 9):
                cp(CPr[:, :, s_], CPr[:, :, s_ - 1], P, P); cp(CPi[:, :, s_], CPi[:, :, s_ - 1], P, P)
                csq(CPr[:, :, s_], CPi[:, :, s_])
            ts(CPin[:], CPi[:], -1.0, None, M, None, P, P)
            ts(g("numr"), g("pr"), -1.0, None, A, None, P, P)
            tt(g("t1"), g("lamr"), g("lamr"), M, P, P); tt(g("t2"), g("lami"), g("lami"), M, P, P)
            tt(g("den"), g("t1"), g("t2"), A, P, P)
            sc.op("dve", lambda e: e.reciprocal(out=g("rden"), in_=g("den")), P, P)
            tt(g("t1"), g("numr"), g("lamr"), M, P, P); tt(g("t2"), g("pi"), g("lami"), M, P, P)
            tt(g("t1"), g("t1"), g("t2"), A, P, P); tt(g("cr"), g("t1"), g("rden"), M, P, P)
            tt(g("t1"), g("pi"), g("lamr"), M, P, P); tt(g("t2"), g("numr"), g("lami"), M, P, P)
            tt(g("t1"), g("t1"), g("t2"), SUB, P, P); tt(g("ci"), g("t1"), g("rden"), M, P, P)
            bc = lambda ap: ap.to_broadcast([128, 16, 16])
            crb, cib = bc(T_["cr"][:].unsqueeze(2)), bc(T_["ci"][:].unsqueeze(2))
            tt(tA[:], Bre[:], crb, M, P, P); tt(tB[:], Bim[:], cib, M, P, P); tt(bbr[:], tA[:], tB[:], SUB, P, P)
            tt(tA[:], Bim[:], crb, M, P, P); tt(tB[:], Bre[:], cib, M, P, P); tt(bbi[:], tA[:], tB[:], A, P, P)
            cp(bbrb[:], bbr[:], P, P); cp(bbib[:], bbi[:], P, P)
            Wr4 = Wr[:].rearrange("p a (i q) -> p a i q", q=16); Wi4 = Wi[:].rearrange("p a (i q) -> p a i q", q=16)
            for i in range(8):
                pwr, pwi = bc(PWr[:, :, 7 - i:8 - i]), bc(PWi[:, :, 7 - i:8 - i])
                tt(tA[:], bbr[:], pwr, M, P, P); tt(tB[:], bbi[:], pwi, M, P, P); tt(Wr4[:, :, i, :], tA[:], tB[:], SUB, P, P)
                tt(tA[:], bbr[:], pwi, M, P, P); tt(tB[:], bbi[:], pwr, M, P, P); tt(Wi4[:, :, i, :], tA[:], tB[:], A, P, P)
            EEr4 = EEr[:].rearrange("p a (k q) -> p a k q", q=16); EEi4 = EEi[:].rearrange("p a (k q) -> p a k q", q=16)
            for k in range(9):
                pwr, pwi = bc(PWr[:, :, k:k + 1]), bc(PWi[:, :, k:k + 1])
                tt(tA[:], Cre[:], pwr, M, P, P); tt(tB[:], Cim[:], pwi, M, P, P); tt(EEr4[:, :, k, :], tA[:], tB[:], SUB, P, P)
                tt(tA[:], Cre[:], pwi, M, P, P); tt(tB[:], Cim[:], pwr, M, P, P); tt(EEi4[:, :, k, :], tA[:], tB[:], A, P, P)
            EEm5 = EEm[:].rearrange("p (a b) c d -> p a b c d", b=2)
            for g2 in range(2):
                ts(EEm5[:, :, g2, 0, :], EEr[:], mask[:, g2:g2 + 1], None, M, None, P, P)
                ts(EEm5[:, :, g2, 1, :], EEi[:], mask[:, 2 + g2:3 + g2], None, M, None, P, P)
            ms(BsT[:].rearrange("p a b c -> p (a b c)"), 0.0, P, eng="pool")
            for p_ in range(16):
                for plane, W in ((0, Wr), (1, Wi)):
                    slot = (2 * p_ + plane) % 4
                    tr(ptf[:, slot, :], W[:, p_, :], C.identF[:], P + ["identF"], ["ptf%d" % slot])
                    cp(BsT[:, 2 * p_, plane, 0:64], ptf[:, slot, 0:64], ["ptf%d" % slot], P, eng="act")
                    cp(BsT[:, 2 * p_ + 1, plane, 64:128], ptf[:, slot, 64:128], ["ptf%d" % slot], P)
            for g_ in range(32):
                p_ = g_ // 2
                mm(pk[:, g_ % 4, :], bbrb[:, p_, :], EEm[:, g_, 0, 0:128], True, False, P, ["pk"])
                mm(pk[:, g_ % 4, :], bbib[:, p_, :], EEm[:, g_, 1, 0:128], False, True, P, ["pk"])
                if g_ % 4 == 3:
                    cp(Kall[:, g_ - 3:g_ + 1, :], pk[:], ["pk"], P, eng="act")
            ms(T32[:].rearrange("p a b -> p (a b)"), 0.0, P, eng="pool")
            for i in range(8):
                dma(T32[16 * i:16 * i + 16, :, 16 * i:128], Kall[0:16, :, 0:128 - 16 * i], P, P)
            for g_ in range(32):
                stt(Tb[:, g_, :], C.identF[:], dcol[:, g_:g_ + 1], T32[:, g_, :], M, A, P + ["identF"], P)
            sc.barrier()
        with ExitStack() as pm:
            sbm = lambda name, shape, dt=F32: pm.enter_context(nc.sbuf_tensor(uniq(name), list(shape), dt))
            psm_ = lambda name, shape, dt=F32: pm.enter_context(nc.psum_tensor(uniq(name), list(shape), dt))
            uch = [sbm("uch%d" % i, [128, 8, 128]) for i in range(2)]
            uchb = [sbm("uchb%d" % i, [128, 8, 128], BF16) for i in range(2)]
            UT = sbm("UT", [128, 8, 512], BF16)
            XA = [sbm("XAr", [128, 4, 513]), sbm("XAi", [128, 4, 513])]
            XB = [sbm("XBr", [128, 4, 513]), sbm("XBi", [128, 4, 513])]
            Xbf = [sbm("Xbfr", [128, 4, 513], BF16), sbm("Xbfi", [128, 4, 513], BF16)]
            ysb = [sbm("ysb%d" % i, [128, 8, 128]) for i in range(2)]
            ptu = psm_("ptu", [128, 8, 128], BF16)
            psS = [psm_("psS%d" % i, [128, 512]) for i in range(2)]
            psY = [psm_("psY%d" % i, [128, 4, 128]) for i in range(2)]
            for pl in range(2):
                ms(XA[pl][:, :, 0:1], 0.0, ["XA"]); ms(XB[pl][:, :, 0:1], 0.0, ["XB"])
            projv = C.proj.rearrange("(c j) n -> c j n", j=8)
            yv = C.yscr.rearrange("(c j) n -> c j n", j=8)
            for b in range(4):
                for ct in range(4):
                    sl = ct % 2
                    dma(uch[sl][:], projv[ct * 128:(ct + 1) * 128, :, b * 128:(b + 1) * 128], [], ["uch%d" % sl])
                    cp(uchb[sl][:].rearrange("c g (j q) -> c j g q", q=16),
                       uch[sl][:].rearrange("c j (g q) -> c j g q", q=16), ["uch%d" % sl], ["uchb%d" % sl], eng="pool")
                    for gl in range(8):
                        tr(ptu[:, gl, :], uchb[sl][:, gl, :], C.identB[:],
                           ["uchb%d" % sl, "identB"], ["ptu"])
                    cp(UT[:, :, ct * 128:(ct + 1) * 128], ptu[:], ["ptu"], ["UT"], eng="act")
                for pl_ in range(4):
                    g0 = 8 * b + 2 * pl_
                    for plane in range(2):
                        s_ = (2 * pl_ + plane) % 2
                        mm(psS[s_][:], BsT[:, g0, plane, :], UT[:, 2 * pl_, :], True, False, ["UT"], ["psS%d" % s_])
                        mm(psS[s_][:], BsT[:, g0 + 1, plane, :], UT[:, 2 * pl_ + 1, :], False, True, ["UT"], ["psS%d" % s_])
                        cp(XA[plane][:, pl_, 1:513], psS[s_][:], ["psS%d" % s_], ["XA"], eng=("act" if plane else "dve"))
                src, dst, sn, dn = XA, XB, "XA", "XB"
                for s_ in range(9):
                    sh = 1 << s_
                    for pl_ in range(4):
                        p_ = 4 * b + pl_
                        pr_, pi_, pin_ = CPr[:, p_, s_:s_ + 1], CPi[:, p_, s_:s_ + 1], CPin[:, p_, s_:s_ + 1]
                        lo = lambda t: t[:, pl_, 1:513 - sh]
                        hi = lambda t: t[:, pl_, 1 + sh:513]
                        stt(hi(dst[0]), lo(src[0]), pr_, hi(src[0]), M, A, [sn], [dn + "r%d" % pl_])
                        stt(hi(dst[1]), lo(src[1]), pr_, hi(src[1]), M, A, [sn], [dn + "i%d" % pl_])
                    for pl_ in range(4):
                        p_ = 4 * b + pl_
                        pr_, pi_, pin_ = CPr[:, p_, s_:s_ + 1], CPi[:, p_, s_:s_ + 1], CPin[:, p_, s_:s_ + 1]
                        lo = lambda t: t[:, pl_, 1:513 - sh]
                        hi = lambda t: t[:, pl_, 1 + sh:513]
                        stt(hi(dst[0]), lo(src[1]), pin_, hi(dst[0]), M, A, [sn, dn + "r%d" % pl_], [dn + "r%d" % pl_, dn])
                        stt(hi(dst[1]), lo(src[0]), pi_, hi(dst[1]), M, A, [sn, dn + "i%d" % pl_], [dn + "i%d" % pl_, dn])
                    cp(dst[0][:, :, 1:1 + sh], src[0][:, :, 1:1 + sh], [sn], [dn], eng="pool")
                    cp(dst[1][:, :, 1:1 + sh], src[1][:, :, 1:1 + sh], [sn], [dn], eng="pool")
                    src, dst, sn, dn = dst, src, dn, sn
                cp(Xbf[0][:], src[0][:], [sn], ["Xbf"], eng="act")
                cp(Xbf[1][:], src[1][:], [sn], ["Xbf"], eng="act")
                for ct in range(4):
                    sl = ct % 2
                    for half in range(2):
                        for glh in range(4):
                            gl = half * 4 + glh
                            g_ = 8 * b + gl
                            pl_ = gl // 2
                            o = psY[half][:, glh, :]
                            mm(o, UT[:, gl, ct * 128:(ct + 1) * 128], Tb[:, g_, :], True, False, ["UT"], ["psY%d" % half])
                            mm(o, Xbf[0][:, pl_, ct * 128:ct * 128 + 128], EEm[:, g_, 0, 16:144], False, False, ["Xbf"], ["psY%d" % half])
                            mm(o, Xbf[1][:, pl_, ct * 128:ct * 128 + 128], EEm[:, g_, 1, 16:144], False, True, ["Xbf"], ["psY%d" % half])
                        cp(ysb[sl][:, :, half * 64:(half + 1) * 64].rearrange("c j (g p) -> c j g p", p=16),
                           psY[half][:].rearrange("c g (j p) -> c j g p", p=16),
                           ["psY%d" % half], ["ysb%d" % sl], eng=("act" if half else "dve"))
                    dma(yv[ct * 128:(ct + 1) * 128, :, b * 128:(b + 1) * 128], ysb[sl][:], ["ysb%d" % sl], ["yscr"])
            sc.barrier()
        with ExitStack() as pq:
            sbq = lambda name, shape, dt=F32: pq.enter_context(nc.sbuf_tensor(uniq(name), list(shape), dt))
            psq = lambda name, shape, dt=F32: pq.enter_context(nc.psum_tensor(uniq(name), list(shape), dt))
            yt = [sbq("yt%d" % i, [128, 512]) for i in range(4)]
            g1 = [sbq("g1_%d" % i, [128, 512]) for i in range(2)]; g2t = [sbq("g2t%d" % i, [128, 512]) for i in range(2)]
            zb = [sbq("zb%d" % i, [128, 512], BF16) for i in range(2)]
            zT = [sbq("zT%d" % i, [128, 4, 512], BF16) for i in range(2)]
            sgt = [sbq("sgt%d" % i, [128, 512], BF16) for i in range(2)]
            ptz = psq("ptz", [128, 4, 128], BF16)
            psG = [psq("psG%d" % i, [128, 512]) for i in range(2)]
            s5T = sbq("s5T", [128, 32, 4, 128], BF16)
            KG = float(2.0 * np.sqrt(2.0 / np.pi))

            def q0(t):
                dma(yt[t % 4][:], C.yscr[t * 128:(t + 1) * 128, :], ["yscr"], ["yt%d" % (t % 4)])

            def q1(t):
                y_, Y, G1 = yt[t % 4], "yt%d" % (t % 4), "g1_%d" % (t % 2)
                g_ = g1[t % 2]
                tt(g_[:], y_[:], y_[:], M, [Y], [G1], eng="pool")
                ts(g_[:], g_[:], 0.044715, 1.0, M, A, [G1], [G1])
                tt(g_[:], g_[:], y_[:], M, [G1, Y], [G1])

            def q2(t):
                act(g2t[t % 2][:], g1[t % 2][:], AF.Sigmoid, ["g1_%d" % (t % 2)], ["g2t%d" % (t % 2)], scale=KG)

            def q3(t):
                sl = t % 2
                tt(zb[sl][:], g2t[sl][:], yt[t % 4][:], M, ["g2t%d" % sl, "yt%d" % (t % 4)], ["zb%d" % sl])

            def q4(t):
                sl = t % 2
                zs = (t // 4) % 2
                for kc in range(4):
                    tr(ptz[:, kc, :], zb[sl][:, kc * 128:(kc + 1) * 128], C.identB[:], ["zb%d" % sl, "identB"], ["ptz"])
                cp(zT[zs][:, :, (t % 4) * 128:(t % 4 + 1) * 128], ptz[:], ["ptz"], ["zT%d" % zs], eng="act")

            def q5(t):
                if t % 4 != 3:
                    return
                zs = (t // 4) % 2
                tg = t // 4
                for ncx in range(4):
                    s_ = ncx % 2
                    for kc in range(4):
                        mm(psG[s_][:], wglu[:, kc, ncx * 128:(ncx + 1) * 128], zT[zs][:, kc, :], kc == 0, kc == 3,
                           ["wglu", "zT%d" % zs], ["psG%d" % s_])
                    act(sgt[s_][:], psG[s_][:], AF.Sigmoid, ["psG%d" % s_, "bgl"], ["sgt%d" % s_], bias=bgl[:, ncx:ncx + 1])
                    tt(s5T[:, tg * 4:(tg + 1) * 4, ncx, :], zT[zs][:, ncx, :].rearrange("p (t j) -> p t j", j=128),
                       sgt[s_][:].rearrange("p (t j) -> p t j", j=128), M,
                       ["zT%d" % zs, "sgt%d" % s_], ["s5T"])

            qst = [q0, q1, q2, q3, q4, q5]
            for step in range(NT + len(qst) - 1):
                for k in reversed(range(len(qst))):
                    t = step - k
                    if 0 <= t < NT:
                        qst[k](t)
            dma(C.s5T_d, s5T[:], ["s5T"], ["s5T_d"])
            sc.barrier()


SLOPES = [2.0 ** (-(h + 1)) for h in range(8)]
NEGV = -30000.0


def nsa_consts():
    import ml_dtypes
    bf = ml_dtypes.bfloat16
    kl = np.arange(128)[:, None].astype(np.float64)
    tl = np.arange(512)[None, :].astype(np.float64)
    diffS = (tl - kl).astype(np.float32)
    diffC = (tl - 16 * kl - 31).astype(np.float32)
    negm = np.zeros((128, 19, 512), np.float32)
    for i in range(5):
        negm[:, i, :] = np.where(diffC + 512 * i >= 0, 0.0, NEGV)
    for m in range(4):
        negm[:, 5 + m, :] = np.where(diffS - 128 * m >= 0, 0.0, NEGV)
    for i, dl in enumerate((2, 1, 0, -1, -2, -3)):
        dd = diffS + 128 * dl
        negm[:, 9 + i, :] = np.where((dd >= 0) & (dd < 256), 0.0, NEGV)
    for i in range(4):
        negm[:, 15 + i, :] = negm[:, i, :]
        negm[127, 15 + i, :] = NEGV
    t = (np.arange(32)[None, :, None] * 128 + np.arange(128)[:, None, None])
    cur = t // 64
    j = np.arange(64)[None, None, :]
    valid = j <= cur
    forced = (j == 0) | (j == cur) | (j == cur - 1)
    mvadd = np.zeros((128, 32, 2, 64), np.float32)
    mvadd[:, :, 0, :] = valid
    mvadd[:, :, 1, :] = np.where(valid, forced * 1000.0, -1.0)
    ebc = np.zeros((128, 32, 128), np.float32)
    for kc in range(32):
        ebc[2 * kc, kc, :64] = 1.0
        ebc[2 * kc + 1, kc, 64:] = 1.0
    cmp_start = np.arange(255) * 16
    sel_start = np.arange(64) * 64
    ov = ((cmp_start[:, None] <= sel_start[None, :] + 63) & (cmp_start[:, None] + 31 >= sel_start[None, :]))
    ovp = np.zeros((256, 64), np.float32); ovp[:255] = ov
    blk = np.zeros((128, 128), np.float32); blk[:64, :64] = 1.0; blk[64:, 64:] = 1.0
    tlv = np.arange(512)
    auxl = np.zeros((128, 128), np.float32)
    auxl[[0, 1], :] = 1.0
    auxr = np.zeros((128, 8, 512), np.float32)
    for h in range(8):
        for base in (0,):
            auxr[base, h, :] = -SLOPES[h] * 16.0 * (tlv // 16)
            auxr[base + 1, h, :] = -SLOPES[h] * (tlv % 16)
    klv = np.arange(128).astype(np.float64)
    biasS = np.zeros((128, 8, 35), np.float32)
    biasC = np.zeros((128, 8, 8, 2), np.float32)
    for h in range(8):
        for di in range(35):
            biasS[:, h, di] = SLOPES[h] * (klv - 128.0 * (di - 3))
        for Q in range(8):
            for ct in range(2):
                biasC[:, h, Q, ct] = SLOPES[h] * (16.0 * klv + 31.0 - (512.0 * Q - 2048.0 * ct))
    return {
        "n_auxl": auxl.astype(bf), "n_auxr": auxr.astype(bf), "n_biasS": biasS, "n_biasC": biasC,
        "n_diffS": diffS, "n_diffC": diffC, "n_negm": negm.astype(bf), "n_mvadd": mvadd.astype(bf),
        "n_ebc": ebc.astype(bf), "n_ovl": np.ascontiguousarray(ovp.reshape(2, 128, 64).transpose(1, 0, 2)),
        "n_blk64": blk,
    }


def nsa_inputs(inp):
    f = lambda a: np.ascontiguousarray(a, dtype=np.float32)
    pe = inp["cmp_pe"][0]
    return {
        "n_qg": f(np.tile(inp["q_norm_gain"][0], 8)[None, :]),
        "n_kg1": f(np.tile(inp["k_norm_gain"][0, 1], 2)[None, :]),
        "n_kg2": f(np.tile(inp["k_norm_gain"][0, 2], 2)[None, :]),
        "n_kg0": f(np.tile(inp["k_norm_gain"][0, 0], 2)[:, None]),
        "n_peT": f(np.stack([pe[j].reshape(16, 2, 64).transpose(1, 2, 0).reshape(128, 16) for j in range(2)], axis=1)),
        "n_b1T": f(inp["cmp_b1"][0].reshape(2, 2, 128).transpose(2, 0, 1)),
        "n_w1": f(inp["cmp_w1"][0]),
        "n_w2": f(inp["cmp_w2"][0]),
        "n_b2k": f(np.tile(inp["cmp_b2"][0, 0], 2)[:, None]),
        "n_b2v": f(inp["cmp_b2"][0, 1][None, :]),
        "n_mask01": np.concatenate([np.repeat([[1.0, 0.0]], 64, 0), np.repeat([[0.0, 1.0]], 64, 0)]).astype(np.float32),
        **nsa_consts(),
    }


def phase_nsa(C):
    nc, sc = C.nc, C.sc
    H = mk_helpers(sc)
    tt, ts, stt, act, cp, ms, mm, tr, dma = H.tt, H.ts, H.stt, H.act, H.cp, H.ms, H.mm, H.tr, H.dma
    M, A, SUB = ALU.mult, ALU.add, ALU.subtract
    din = C.din
    d_qg = din("n_qg", [1, 512]); d_kg1 = din("n_kg1", [1, 128]); d_kg2 = din("n_kg2", [1, 128]); d_kg0 = din("n_kg0", [128, 1])
    d_peT = din("n_peT", [128, 2, 16]); d_b1T = din("n_b1T", [128, 2, 2]); d_w1 = din("n_w1", [2, 2048, 256])
    d_m01 = din("n_mask01", [128, 2]); d_w2 = din("n_w2", [2, 256, 64]); d_b2k = din("n_b2k", [128, 1]); d_b2v = din("n_b2v", [1, 64])
    d_auxl = din("n_auxl", [128, 128], BF16); d_auxr = din("n_auxr", [128, 8, 512], BF16)
    d_biasS = din("n_biasS", [128, 8, 35]); d_biasC = din("n_biasC", [128, 8, 8, 2])
    d_negm = din("n_negm", [128, 19, 512], BF16); d_mvadd = din("n_mvadd", [128, 32, 2, 64], BF16)
    d_ebc = din("n_ebc", [128, 32, 128], BF16); d_ovl = din("n_ovl", [128, 2, 64]); d_blk = din("n_blk64", [128, 128])
    proj = C.proj
    KG = float(2.0 * np.sqrt(2.0 / np.pi))
    with ExitStack() as ph:
        sbp = lambda name, shape, dt=F32: ph.enter_context(nc.sbuf_tensor(uniq(name), list(shape), dt))
        qT = sbp("qT", [128, 4, S], BF16)
        ksT2 = sbp("ksZ", [128, 2, 2, S], BF16)
        kwT2 = sbp("kwZ", [128, 2, 2, S], BF16)
        vs_aug = sbp("vs_aug", [128, 32, 2, 65], BF16)
        vw_aug = sbp("vw_aug", [128, 32, 2, 65], BF16)
        gn = sbp("gn", [128, 32, 24])
        kcT2 = sbp("kcZ", [128, 2, 2, 256], BF16)
        vcA = sbp("vcA", [128, 2, 2, 65], BF16)
        ovl = sbp("ovl", [128, 2, 64], BF16)
        with ExitStack() as p1:
            sb1 = lambda name, shape, dt=F32: p1.enter_context(nc.sbuf_tensor(uniq(name), list(shape), dt))
            ps1 = lambda name, shape, dt=F32: p1.enter_context(nc.psum_tensor(uniq(name), list(shape), dt))
            qg = sb1("qg", [128, 512]); kg1 = sb1("kg1", [128, 128]); kg2 = sb1("kg2", [128, 128])
            kg0 = sb1("kg0", [128, 1]); b2k = sb1("b2k", [128, 1]); b2v = sb1("b2v", [1, 64]); b2vb = sb1("b2vb", [1, 64], BF16)
            onesb = sb1("onesb", [1, 128], BF16)
            blk = sb1("blk", [128, 128])
            ovf = sb1("ovf", [128, 2, 64])
            pt = [sb1("pt%d" % i, [128, 1304]) for i in range(4)]
            sqs = [sb1("sq%d" % i, [128, 768]) for i in range(2)]
            st12s = [sb1("st12_%d" % i, [128, 12]) for i in range(3)]; st12bs = [sb1("st12b%d" % i, [128, 12]) for i in range(3)]
            qn = sb1("qn", [128, 512]); qnbs = [sb1("qnb%d" % i, [128, 512], BF16) for i in range(2)]
            kn = sb1("kn", [128, 256]); knbs = [sb1("knb%d" % i, [128, 2, 2, 2, 128], BF16) for i in range(2)]
            ptk = ps1("ptk", [128, 8, 128], BF16)
            ptr = ps1("ptr", [128, 8, 128], BF16)
            dma(qg[:], d_qg.partition_broadcast(128), [], ["qg"]); dma(kg1[:], d_kg1.partition_broadcast(128), [], ["kg"])
            dma(kg2[:], d_kg2.partition_broadcast(128), [], ["kg"]); dma(kg0[:], d_kg0, [], ["kg0"])
            dma(b2k[:], d_b2k, [], ["b2k"]); dma(b2v[:], d_b2v, [], ["b2v"]); dma(blk[:], d_blk, [], ["blk"])
            dma(ovf[:], d_ovl, [], ["ovf"])
            cp(ovl[:], ovf[:], ["ovf"], ["ovl"])
            cp(b2vb[:], b2v[:], ["b2v"], ["b2vb"])
            ms(onesb[:], 1.0, ["onesb"])
            ts(qg[:], qg[:], 0.125, None, M, None, ["qg"], ["qg"])
            ms(vs_aug[:, :, :, 64:65], 1.0, ["vs_aug"], eng="pool"); ms(vw_aug[:, :, :, 64:65], 1.0, ["vw_aug"], eng="pool")
            for i_ in range(2):
                ms(knbs[i_][:].rearrange("p a b c d -> p (a b c d)"), 0.0, ["knb%d" % i_])
            v64 = lambda ap: ap.rearrange("p (a d) -> p a d", d=64)

            def d0(t):
                dma(pt[t % 4][:], proj[t * 128:(t + 1) * 128, 512:1816], [], ["pt%d" % (t % 4)])

            def d1(t):
                p_, PT, s3 = pt[t % 4], "pt%d" % (t % 4), t % 3
                SQ, ST = "sq%d" % (t % 2), "st12_%d" % s3
                sq_ = sqs[t % 2]
                tt(sq_[:, 0:512], p_[:, 0:512], p_[:, 0:512], M, [PT], [SQ], eng="pool")
                tt(sq_[:, 512:640], p_[:, 768:896], p_[:, 768:896], M, [PT], [SQ], eng="pool")
                tt(sq_[:, 640:768], p_[:, 1024:1152], p_[:, 1024:1152], M, [PT], [SQ], eng="pool")
                cp(vs_aug[:, t, :, 0:64], v64(p_[:, 896:1024]), [PT], ["vs_aug"], eng="pool")
                cp(vw_aug[:, t, :, 0:64], v64(p_[:, 1152:1280]), [PT], ["vw_aug"], eng="pool")
                act(gn[:, t, :], p_[:, 1280:1304], AF.Sigmoid, [PT], ["gn"])

            def d2(t):
                s3 = t % 3
                SQ, ST = "sq%d" % (t % 2), "st12_%d" % s3
                sc.op("dve", lambda e: e.tensor_reduce(out=st12s[s3][:], in_=v64(sqs[t % 2][:]), axis=AX.X, op=ALU.add), [SQ], [ST])
                ts(st12s[s3][:], st12s[s3][:], 1.0 / 64, RMS_EPS, M, A, [ST], [ST])
                act(st12bs[s3][:], st12s[s3][:], AF.Sqrt, [ST], [ST + "b"])

            def d3(t):
                p_, PT, s3, s2 = pt[t % 4], "pt%d" % (t % 4), t % 3, t % 2
                ST = "st12_%d" % s3
                st_ = st12s[s3]
                sc.op("dve", lambda e: e.reciprocal(out=st_[:], in_=st12bs[s3][:]), [ST + "b"], [ST])
                tt(v64(qn[:]), v64(p_[:, 0:512]), st_[:, 0:8].unsqueeze(2).to_broadcast([128, 8, 64]), M, [PT, ST], ["qn"])
                tt(v64(kn[:, 0:128]), v64(p_[:, 768:896]), st_[:, 8:10].unsqueeze(2).to_broadcast([128, 2, 64]), M, [PT, ST], ["kn"])
                tt(v64(kn[:, 128:256]), v64(p_[:, 1024:1152]), st_[:, 10:12].unsqueeze(2).to_broadcast([128, 2, 64]), M, [PT, ST], ["kn"])
                tt(qnbs[s2][:], qn[:], qg[:], M, ["qn", "qg"], ["qnb%d" % s2])
                for w_, kg in ((0, kg1), (1, kg2)):
                    for par in range(2):
                        tt(knbs[s2][:, w_, :, par, par * 64:(par + 1) * 64], v64(kn[:, w_ * 128:(w_ + 1) * 128]), v64(kg[:]), M,
                           ["kn", "kg"], ["knb%d" % s2], eng=("pool" if par else "dve"))

            def d4(t):
                s2 = t % 2
                for c4 in range(4):
                    tr(ptr[:, c4, :], qnbs[s2][:, c4 * 128:(c4 + 1) * 128], C.identB[:], ["qnb%d" % s2, "identB"], ["ptr"])
                for w_ in range(2):
                    for hk in range(2):
                        for par in range(2):
                            tr(ptk[:, 4 * w_ + 2 * hk + par, :], knbs[s2][:, w_, hk, par, :], C.identB[:],
                               ["knb%d" % s2, "identB"], ["ptk"])
                cp(qT[:, :, t * 128:(t + 1) * 128], ptr[:, 0:4, :], ["ptr"], ["qT"], eng="act")
                cp(ksT2[:, :, :, t * 128:(t + 1) * 128], ptk[:, 0:4, :].rearrange("p (a b) j -> p a b j", b=2), ["ptk"], ["ksT2"], eng="act")
                cp(kwT2[:, :, :, t * 128:(t + 1) * 128], ptk[:, 4:8, :].rearrange("p (a b) j -> p a b j", b=2), ["ptk"], ["kwT2"], eng="act")

            dst = [d0, d1, d2, d3, d4]
            for step in range(NT + len(dst) - 1):
                for k in reversed(range(len(dst))):
                    t = step - k
                    if 0 <= t < NT:
                        dst[k](t)
            sc.barrier()
        import os
        NSTOP = int(os.environ.get('NSA_STOP', '3'))
        if NSTOP < 2:
            return
        with ExitStack() as p2:
            sb2 = lambda name, shape, dt=F32: p2.enter_context(nc.sbuf_tensor(uniq(name), list(shape), dt))
            ps2 = lambda name, shape, dt=F32: p2.enter_context(nc.psum_tensor(uniq(name), list(shape), dt))
            PS = sb2("PS", [128, 4, 2048], BF16)
            pc = [sb2("pc%d" % i, [128, 2, 256]) for i in range(2)]
            pcb = [sb2("pcb%d" % i, [128, 4, 2, 64], BF16) for i in range(2)]
            w1st = sb2("w1st", [128, 16, 256])
            w1b = sb2("w1b", [128, 2, 16, 256], BF16)
            w2f = sb2("w2f", [128, 2, 2, 64]); w2kd = sb2("w2kd", [128, 2, 2, 64], BF16); w2v = sb2("w2v", [128, 2, 64], BF16)
            peT = sb2("peT", [128, 2, 16]); peTb = sb2("peTb", [128, 2, 16], BF16)
            b1T = sb2("b1T", [128, 2, 2]); bias1 = sb2("bias1", [128, 2, 2])
            m01 = sb2("m01", [128, 2]); kg0 = sb2("kg0b", [128, 1]); b2k = sb2("b2kb", [128, 1]); b2v = sb2("b2v2", [1, 64]); b2vb = sb2("b2vb2", [1, 64], BF16)
            onesb = sb2("onesb2", [1, 128], BF16); blk = sb2("blk2", [128, 128])
            hx = sb2("hx", [128, 256]); hg = sb2("hg", [128, 256]); hs = sb2("hs", [128, 256])
            hidb = sb2("hidb", [128, 2, 256], BF16)
            kcf = sb2("kcf", [128, 256]); kcs = sb2("kcs", [128, 256]); kr = sb2("kr", [128, 256]); kr2 = sb2("kr2", [128, 256])
            pt2 = ps2("pt2", [128, 4, 128], BF16)
            pb = ps2("pb", [128, 4])
            ph_ = [ps2("ph%d" % i, [128, 256]) for i in range(2)]
            pv = ps2("pv", [128, 2, 64])
            dma(kg0[:], d_kg0, [], ["kg0"]); dma(b2k[:], d_b2k, [], ["b2k"]); dma(b2v[:], d_b2v, [], ["b2v"])
            dma(m01[:], d_m01, [], ["kg0"])
            dma(blk[:], d_blk, [], ["blk"]); dma(peT[:], d_peT, [], ["peT"]); dma(b1T[:], d_b1T, [], ["b1T"])
            cp(b2vb[:], b2v[:], ["b2v"], ["b2vb"]); ms(onesb[:], 1.0, ["onesb"]); cp(peTb[:], peT[:], ["peT"], ["peTb"])
            dma(w2f[:], d_w2.rearrange("j (hc p) d -> p j hc d", p=128), [], ["w2f"])
            for dup in range(2):
                pass
            w2kd_v = w2kd
            for dup in range(2):
                cp(w2kd[:, :, dup, :], w2f[:, 0, :, :], ["w2f"], ["w2kd"])
            cp(w2v[:], w2f[:, 1, :, :], ["w2f"], ["w2v"])
            for j in range(2):
                dma(w1st[:], d_w1[j].rearrange("(kk p) h -> p kk h", p=128), [], ["w1st"])
                cp(w1b[:, j, :, :], w1st[:], ["w1st"], ["w1b"], eng=("pool" if j else "dve"))
            projp = proj.rearrange("(m l) n -> m l n", l=2)
            for mt in range(16):
                sl = mt % 2
                dma(pc[sl][:], projp[mt * 128:(mt + 1) * 128, :, 1024:1280], [], ["pc%d" % sl])
                cp(pcb[sl][:].rearrange("m c l d -> m l c d"), pc[sl][:].rearrange("m l (c d) -> m l c d", d=64),
                   ["pc%d" % sl], ["pcb%d" % sl], eng="pool")
                for cb in range(4):
                    tr(pt2[:, cb, :], pcb[sl][:, cb, :, :].rearrange("m l d -> m (l d)"), C.identB[:],
                       ["pcb%d" % sl, "identB"], ["pt2"])
                cp(PS[:, :, mt * 128:(mt + 1) * 128], pt2[:], ["pt2"], ["PS"], eng="act")
            for j in range(2):
                for hc in range(2):
                    for kk in range(16):
                        mm(pb[:, 2 * j + hc:2 * j + hc + 1], w1b[:, j, kk, hc * 128:(hc + 1) * 128], peTb[:, j, kk:kk + 1],
                           kk == 0, kk == 15, ["w1b", "peTb"], ["pb"])
            tt(bias1[:].rearrange("p a b -> p (a b)"), pb[:], b1T[:].rearrange("p a b -> p (a b)"), A, ["pb", "b1T"], ["bias1"])
            ms(hidb[:, :, 255:256], 0.0, ["hidb"])
            ms(vcA[:].rearrange("p a b c -> p (a b c)"), 0.0, ["vcA"])
            for j in range(2):
                for hk in range(2):
                    cb = 2 * j + hk
                    for hc in range(2):
                        for kk in range(16):
                            mm(ph_[hc][:, 0:255], w1b[:, j, kk, hc * 128:(hc + 1) * 128], PS[:, cb, kk:kk + 8 * 254 + 1:8],
                               kk == 0, kk == 15, ["w1b", "PS"], ["ph%d" % hc])
                        act(hx[:, 0:255], ph_[hc][:, 0:255], AF.Identity, ["ph%d" % hc, "bias1"], ["hx"], bias=bias1[:, j, hc:hc + 1])
                        tt(hg[:, 0:255], hx[:, 0:255], hx[:, 0:255], M, ["hx"], ["hg"])
                        ts(hg[:, 0:255], hg[:, 0:255], 0.044715, 1.0, M, A, ["hg"], ["hg"])
                        tt(hg[:, 0:255], hg[:, 0:255], hx[:, 0:255], M, ["hg", "hx"], ["hg"])
                        act(hs[:, 0:255], hg[:, 0:255], AF.Sigmoid, ["hg"], ["hs"], scale=KG)
                        tt(hidb[:, hc, 0:255], hs[:, 0:255], hx[:, 0:255], M, ["hs", "hx"], ["hidb"])
                    if j == 0:
                        for hc in range(2):
                            mm(ph_[0][:], w2kd[:, hc, :, :].rearrange("p a d -> p (a d)"), hidb[:, hc, :], hc == 0, hc == 1,
                               ["w2kd", "hidb"], ["ph0"])
                        act(kcf[:], ph_[0][:], AF.Identity, ["ph0", "b2k"], ["kcf"], bias=b2k[:])
                        tt(kcs[:], kcf[:], kcf[:], M, ["kcf"], ["kcs"])
                        mm(ph_[1][:], blk[:], kcs[:], True, True, ["blk", "kcs"], ["ph1"])
                        ts(kr[:], ph_[1][:], 1.0 / 64, RMS_EPS, M, A, ["ph1"], ["kr"])
                        act(kr2[:], kr[:], AF.Sqrt, ["kr"], ["kr2"])
                        sc.op("dve", lambda e: e.reciprocal(out=kr[:], in_=kr2[:]), ["kr2"], ["kr"])
                        tt(kcf[:], kcf[:], kr[:], M, ["kcf", "kr"], ["kcf"])
                        for par in range(2):
                            ts(kcT2[:, hk, par, :], kcf[:], kg0[:, 0:1], m01[:, par:par + 1], M, M, ["kcf", "kg0"], ["kcT2"])
                    else:
                        for ct in range(2):
                            for hc in range(2):
                                mm(pv[:, ct, :], hidb[:, hc, ct * 128:(ct + 1) * 128], w2v[:, hc, :], hc == 0, False,
                                   ["hidb", "w2v"], ["pv"])
                            mm(pv[:, ct, :], onesb[0:1, :], b2vb[0:1, :], False, True, ["onesb", "b2vb"], ["pv"])
                        cp(vcA[:, hk, :, 0:64], pv[:], ["pv"], ["vcA"])
                        ms(vcA[:, hk, :, 64:65], 1.0, ["vcA"])
            sc.barrier()
        if NSTOP < 3:
            return
        with ExitStack() as p3:
            sb3 = lambda name, shape, dt=F32: p3.enter_context(nc.sbuf_tensor(uniq(name), list(shape), dt))
            ps3 = lambda name, shape, dt=F32: p3.enter_context(nc.psum_tensor(uniq(name), list(shape), dt))
            auxl = sb3("auxl", [128, 128], BF16); auxr = sb3("auxr", [128, 8, 512], BF16)
            biasS = sb3("biasS", [128, 8, 35]); biasC = sb3("biasC", [128, 8, 8, 2])
            negm = sb3("negm", [128, 19, 512], BF16); mvadd = sb3("mvadd", [128, 32, 2, 64], BF16)
            ebc = sb3("ebc", [128, 32, 128], BF16)
            tmp = [sb3("tmp%d" % i, [128, 512]) for i in range(4)]
            pex = [sb3("pex%d" % i, [128, 512], BF16) for i in range(4)]
            facw = sb3("facw", [128, 4]); facw2 = sb3("facw2", [128, 4]); otmpw = sb3("otmpw", [128, 4, 64])
            nselb = sb3("nselb", [128, 4, 2, 64], BF16)
            oacc = sb3("oacc", [128, 4, 8, 64])
            oaccb = sb3("oaccb", [128, 4, 512], BF16)
            otmp = sb3("otmp", [128, 4, 64])
            impacc = sb3("impacc", [128, 4, 64]); imp2 = sb3("imp2", [128, 4, 64]); selm = sb3("selm", [128, 4, 64])
            top8 = sb3("top8", [128, 4, 8])
            selT = sb3("selT", [128, 512], BF16)
            nst = sb3("nst", [128, 4, 4, 128], BF16)
            fac = sb3("fac", [128, 4]); fac2 = sb3("fac2", [128, 4])
            psS = [ps3("psS%d" % i, [128, 512]) for i in range(3)]
            psOsL = [ps3("psOs%d" % i, [128, 4, 65]) for i in range(2)]
            psOwL = [ps3("psOw%d" % i, [128, 4, 65]) for i in range(2)]
            ptr3 = ps3("ptr3", [128, 4, 128], BF16)
            dma(auxl[:], d_auxl, [], ["aux"]); dma(auxr[:], d_auxr, [], ["aux"])
            dma(biasS[:], d_biasS, [], ["bias"]); dma(biasC[:], d_biasC, [], ["bias"])
            dma(negm[:], d_negm, [], ["negm"]); dma(mvadd[:], d_mvadd, [], ["mvadd"]); dma(ebc[:], d_ebc, [], ["ebc"])
            cnt = [0]
            NS = 3
            LOOK = 2
            from collections import deque
            pend = deque()

            def submit(front, back):
                if front is not None:
                    front()
                pend.append(back)
                while len(pend) > LOOK:
                    pend.popleft()()

            def flush():
                while pend:
                    pend.popleft()()

            def mm_skip(o, l, rr, st, sp, r, w):
                sc.op("pe", lambda e: e.matmul(o, lhsT=l, rhs=rr, start=st, stop=sp, skip_group_check=True), r, w)

            def chunk_task(kT, kname, hk, h, kcol, Q, bias_ap, nm_idx, use_sel, pvs):
                i = cnt[0] % NS
                cnt[0] += 1
                pb_ = 64 * (h % 2)
                PSN, TMP, PEX = "psS%d" % i, "tmp%d" % i, "pex%d" % i

                def front():
                    mm(psS[i][:], kT[:, hk, h % 2, kcol * 128:(kcol + 1) * 128],
                       qT[:, h // 2, Q * 512:(Q + 1) * 512], True, False, [kname, "qT"], [PSN])
                    mm(psS[i][:], auxl[:, :], auxr[:, h, :], False, not use_sel, ["aux"], [PSN])
                    if use_sel:
                        mm(psS[i][:], ebc[:, kcol, :], selT[:, :], False, True, ["ebc", "selT"], [PSN])

                def back():
                    if nm_idx is not None:
                        tt(tmp[i][:], psS[i][:], negm[:, nm_idx, :], A, [PSN, "negm"], [TMP])
                        act(pex[i][:], tmp[i][:], AF.Exp, [TMP, "bias"], [PEX], bias=bias_ap)
                    else:
                        act(pex[i][:], psS[i][:], AF.Exp, [PSN, "bias"], [PEX], bias=bias_ap)
                    for (view, rhs, rname, pname, first, last) in pvs:
                        for sub in range(4):
                            mm_skip(view(sub), pex[i][:, sub * 128:(sub + 1) * 128], rhs, first and sub == 0, last,
                                    [PEX, rname], [pname])
                submit(front, back)

            def post_cmp(Q, h, g_):
                psOs, psOw = psOsL[g_ % 2], psOwL[g_ % 2]
                PSn, PWn = "psOs%d" % (g_ % 2), "psOw%d" % (g_ % 2)
                psI = psOw[:, :, 0:64]

                def back():
                    ts(fac[:], psOs[:, :, 64], 1e-20, None, ALU.max, None, [PSn], ["fac"])
                    sc.op("dve", lambda e: e.reciprocal(out=fac2[:], in_=fac[:]), ["fac"], ["fac2"])
                    bc4 = fac2[:].unsqueeze(2).to_broadcast([128, 4, 64])
                    if g_ == 0:
                        tt(impacc[:], psI, bc4, M, [PWn, "fac2"], ["impacc"])
                    else:
                        tt(imp2[:], psI, bc4, M, [PWn, "fac2"], ["imp2"])
                        tt(impacc[:], impacc[:], imp2[:], A, ["imp2", "impacc"], ["impacc"])
                    tt(fac[:], fac2[:], gn[:, 4 * Q:4 * Q + 4, h], M, ["fac2", "gn"], ["fac"])
                    tt(oacc[:, :, h, :], psOs[:, :, 0:64], fac[:].unsqueeze(2).to_broadcast([128, 4, 64]), M,
                       [PSn, "fac"], ["oacc"])
                submit(None, back)

            def post_branch(Q, h, br):
                psO, nm_ = (psOsL[h % 2], "psOs%d" % (h % 2)) if br == 1 else (psOwL[h % 2], "psOw%d" % (h % 2))
                fa, fb, ot_ = (fac, fac2, otmp) if br == 1 else (facw, facw2, otmpw)
                fan, fbn, otn = ("fac", "fac2", "otmp") if br == 1 else ("facw", "facw2", "otmpw")

                def back():
                    ts(fa[:], psO[:, :, 64], 1e-20, None, ALU.max, None, [nm_], [fan])
                    sc.op("dve", lambda e: e.reciprocal(out=fb[:], in_=fa[:]), [fan], [fbn])
                    tt(fa[:], fb[:], gn[:, 4 * Q:4 * Q + 4, 8 * br + h], M, [fbn, "gn"], [fan])
                    tt(ot_[:], psO[:, :, 0:64], fa[:].unsqueeze(2).to_broadcast([128, 4, 64]), M, [nm_, fan], [otn])
                    tt(oacc[:, :, h, :], oacc[:, :, h, :], ot_[:], A, [otn, "oacc"], ["oacc"], eng="pool")
                submit(None, back)

            for Q in range(8):
                for hk in range(2):
                    for g_ in range(4):
                        h = 4 * hk + g_
                        cts = [ct for ct in range(2) if 512 * Q - 2048 * ct + 480 >= 0]
                        for ci, ct in enumerate(cts):
                            dl = 512 * Q - 2048 * ct
                            nm = None if dl >= 2063 else (dl // 512 if ct == 0 else 15 + dl // 512)
                            first, last = ci == 0, ci == len(cts) - 1
                            chunk_task(kcT2, "kcT2", hk, h, ct, Q, biasC[:, h, Q, ct:ct + 1], nm, False,
                                       [(lambda sub, g_=g_: psOsL[g_ % 2][:, sub, :], vcA[:, hk, ct, :], "vcA", "psOs%d" % (g_ % 2), first, last),
                                        (lambda sub, g_=g_: psOwL[g_ % 2][:, sub, 0:64], ovl[:, ct, :], "ovl", "psOw%d" % (g_ % 2), first, last)])
                        post_cmp(Q, h, g_)
                    flush()
                    tt(imp2[:], impacc[:], mvadd[:, 4 * Q:4 * Q + 4, 0, :], M, ["impacc", "mvadd"], ["imp2"])
                    tt(imp2[:], imp2[:], mvadd[:, 4 * Q:4 * Q + 4, 1, :], A, ["imp2", "mvadd"], ["imp2"])
                    for sub in range(4):
                        sc.op("dve", lambda e, sub=sub: e.max(out=top8[:, sub, :], in_=imp2[:, sub, :]), ["imp2"], ["top8"])
                    for sub in range(4):
                        ts(selm[:, sub, :], imp2[:, sub, :], top8[:, sub, 7:8], None, ALU.is_ge, None, ["imp2", "top8"], ["selm"])
                    for dup in range(2):
                        ts(nselb[:, :, dup, :], selm[:], -NEGV, NEGV, M, A, ["selm"], ["nselb"])
                    for sub in range(4):
                        tr(ptr3[:, sub, :], nselb[:, sub, :, :].rearrange("p a d -> p (a d)"), C.identB[:],
                           ["nselb", "identB"], ["ptr3"])
                    cp(selT[:], ptr3[:].rearrange("p a b -> p (a b)"), ["ptr3"], ["selT"], eng="act")
                    for g_ in range(4):
                        h = 4 * hk + g_
                        kws = [kc for kc in range(4 * Q - 2, 4 * Q + 4) if kc >= 0]
                        for ci, kc in enumerate(kws):
                            dl = 4 * Q - kc
                            chunk_task(kwT2, "kwT2", hk, h, kc, Q, biasS[:, h, dl + 3:dl + 4], 9 + (2 - dl), False,
                                       [(lambda sub, h=h: psOwL[h % 2][:, sub, :], vw_aug[:, kc, hk, :], "vw_aug", "psOw%d" % (h % 2), ci == 0, ci == len(kws) - 1)])
                        post_branch(Q, h, 2)
                    for g_ in range(4):
                        h = 4 * hk + g_
                        sl_ = SLOPES[h]
                        kcs_ = [kc for kc in range(4 * Q + 4) if not (sl_ * (128 * (4 * Q - kc) - 127) > 115.0)]
                        for ci, kc in enumerate(kcs_):
                            dl = 4 * Q - kc
                            chunk_task(ksT2, "ksT2", hk, h, kc, Q, biasS[:, h, dl + 3:dl + 4], (5 - dl) if dl <= 0 else None, True,
                                       [(lambda sub, h=h: psOsL[h % 2][:, sub, :], vs_aug[:, kc, hk, :], "vs_aug", "psOs%d" % (h % 2), ci == 0, ci == len(kcs_) - 1)])
                        post_branch(Q, h, 1)
                    flush()
                cp(oaccb[:], oacc[:].rearrange("p a h d -> p a (h d)"), ["oacc"], ["oaccb"])
                for sub in range(4):
                    for c4 in range(4):
                        tr(ptr3[:, c4, :], oaccb[:, sub, c4 * 128:(c4 + 1) * 128], C.identB[:], ["oaccb", "identB"], ["ptr3"])
                    cp(nst[:, sub, :, :], ptr3[:], ["ptr3"], ["nst"], eng="act")
                dma(C.nsaT_d[:, Q * 4:(Q + 1) * 4, :, :], nst[:], ["nst"], ["nsaT_d"])
            sc.barrier()


def phase_e(C):
    nc, sc = C.nc, C.sc
    H = mk_helpers(sc)
    tt, ts, stt, act, cp, ms, mm, tr, dma = H.tt, H.ts, H.stt, H.act, H.cp, H.ms, H.mm, H.tr, H.dma
    M, A, SUB = ALU.mult, ALU.add, ALU.subtract
    din = C.din
    d_wa = din("w_branch_a", [512, D]); d_wb = din("w_branch_b", [512, D]); d_wo = din("w_out", [D, D])
    d_g2 = din("g_ffn", [1, D]); d_wr = din("w_router", [D, 64]); d_rb = din("router_bias", [1, 64])
    with ExitStack() as ph:
        sbp = lambda name, shape, dt=F32: ph.enter_context(nc.sbuf_tensor(uniq(name), list(shape), dt))
        psp = lambda name, shape, dt=F32: ph.enter_context(nc.psum_tensor(uniq(name), list(shape), dt))
        modE = sbp("modE", [128, 3 * D])
        dma(modE[:], C.mod_d[:, 2 * D:5 * D], ["mod_d"], ["modB"])
        wa = sbp("wa", [128, 4, D], BF16); wb = sbp("wb", [128, 4, D], BF16); wo = sbp("wo", [128, 8, D], BF16)
        wr = sbp("wr", [128, 8, 64]); rb = sbp("rb", [128, 64]); G2 = sbp("G2", [128, D])
        wrh = sbp("wrh", [128, 8, 64], BF16); wrl = sbp("wrl", [128, 8, 64], BF16)
        with ExitStack() as pw:
            wst = pw.enter_context(nc.sbuf_tensor(uniq("west"), [128, 8, D], F32))
            dma(wst[:, 0:4, :], d_wa.rearrange("(kc p) n -> p kc n", p=128), [], ["west"])
            cp(wa[:], wst[:, 0:4, :], ["west"], ["wa"], eng="pool")
            dma(wst[:, 4:8, :], d_wb.rearrange("(kc p) n -> p kc n", p=128), [], ["west2"])
            cp(wb[:], wst[:, 4:8, :], ["west2"], ["wb"])
            dma(wst[:], d_wo.rearrange("(kc p) n -> p kc n", p=128), ["west2"], ["west", "west2"])
            cp(wo[:, 0:4, :], wst[:, 0:4, :], ["west", "west2"], ["wo"], eng="pool")
            cp(wo[:, 4:8, :], wst[:, 4:8, :], ["west", "west2"], ["wo"])
            dma(wr[:], d_wr.rearrange("(kc p) n -> p kc n", p=128), [], ["wr"])
            dma(rb[:], d_rb.partition_broadcast(128), [], ["rb"])
            cp(wrh[:], wr[:], ["wr"], ["wrh"])
            tt(wrl[:], wr[:], wrh[:], SUB, ["wr", "wrh"], ["wrh"])
            dma(G2[:], d_g2.partition_broadcast(128), [], ["G2"])
            stt(G2[:], modE[:, 2 * D:3 * D], 1.0, G2[:], A, M, ["G2", "modB"], ["G2"])
            sc.barrier()
        NB = 3
        mk = lambda nm, shape, dt=F32, n=NB: [sbp("%s%d" % (nm, i), shape, dt) for i in range(n)]
        s5t = mk("s5t", [128, 4, 128], BF16); nst = mk("nsat", [128, 4, 128], BF16); gm = mk("gm", [128, 2048], BF16)
        xt = mk("xe", [128, D]); mb = mk("mb", [128, D], BF16); mT = mk("mT", [128, 8, 128], BF16)
        x1t = mk("x1t", [128, D], n=4); h2 = mk("h2", [128, D], n=3)
        h2hi = mk("h2hi", [128, D], BF16, n=4); h2lo = mk("h2lo", [128, D], BF16, n=3)
        h2Tb = mk("h2Tb", [128, 8, 128], BF16, n=8); h2Tl = mk("h2Tl", [128, 8, 128], BF16, n=8)
        m1 = sbp("m1", [128, 512]); m2 = sbp("m2", [128, 512]); m3 = sbp("m3", [128, 512])
        junk = sbp("junk2", [128, D], BF16); stat = mk("stat2", [128, 4], n=4); tmpn = sbp("tmpn2", [128, D])
        scs = sbp("scs", [128, 4, 64]); selv = sbp("selv", [128, 4, 64]); eq = sbp("eq", [128, 4, 64]); sel2 = sbp("sel2", [128, 4, 64])
        mx1 = sbp("mx1", [128, 4, 8]); mx2 = sbp("mx2", [128, 4, 8]); gs = sbp("gs", [128, 4, 8]); t8 = sbp("t8", [128, 4, 8])
        gmk = sbp("gmk", [128, 4, 8]); gng = sbp("gng", [128, 4, 8]); den = sbp("den", [128, 8])
        pA = psp("pA", [128, 512]); pB = psp("pB", [128, 512])
        pW = [psp("pW%d" % i, [128, 512]) for i in range(2)]
        pTm = psp("pTm", [128, 8, 128], BF16); pTh = psp("pTh", [128, 8, 128], BF16); pTl = psp("pTl", [128, 8, 128], BF16)
        pL = psp("pL", [128, 4, 64])
        n_ = lambda nm, t: "%s%d" % (nm, t % NB)

        def st0(t):
            sl = t % NB
            dma(s5t[sl][:], C.s5T_d[:, t, :, :], ["s5T_d"], [n_("s5t", t)])
            dma(nst[sl][:], C.nsaT_d[:, t, :, :], ["nsaT_d"], [n_("nsat", t)])
            dma(gm[sl][:], C.gms[t * 128:(t + 1) * 128, :], ["gms"], [n_("gm", t)])
            dma(xt[sl][:], C.x_in[t * 128:(t + 1) * 128, :], [], [n_("xe", t)])

        def st1(t):
            sl = t % NB
            for half in range(2):
                for kc in range(4):
                    mm(pA[:], s5t[sl][:, kc, :], wa[:, kc, half * 512:(half + 1) * 512], kc == 0, kc == 3, [n_("s5t", t), "wa"], ["pA"])
                for kc in range(4):
                    mm(pB[:], nst[sl][:, kc, :], wb[:, kc, half * 512:(half + 1) * 512], kc == 0, kc == 3, [n_("nsat", t), "wb"], ["pB"])
                tt(m1[:], pA[:], gm[sl][:, half * 512:(half + 1) * 512], M, ["pA", n_("gm", t)], ["m1"])
                tt(m2[:], pB[:], gm[sl][:, 1024 + half * 512:1024 + (half + 1) * 512], M, ["pB", n_("gm", t)], ["m2"])
                tt(mb[sl][:, half * 512:(half + 1) * 512], m1[:], m2[:], A, ["m1", "m2"], [n_("mb", t)], eng="pool")

        def st2(t):
            sl = t % NB
            for kc in range(8):
                tr(pTm[:, kc, :], mb[sl][:, kc * 128:(kc + 1) * 128], C.identB[:], [n_("mb", t), "identB"], ["pTm"])
            cp(mT[sl][:], pTm[:], ["pTm"], [n_("mT", t)], eng="act")

        def st3(t):
            sl = t % NB
            s4 = t % 4
            X1, ST = "x1t%d" % s4, "stat%d" % s4
            for half in range(2):
                for kc in range(8):
                    mm(pW[half][:], mT[sl][:, kc, :], wo[:, kc, half * 512:(half + 1) * 512], kc == 0, kc == 7, [n_("mT", t), "wo"], ["pW%d" % half])
                tt(m3[:], pW[half][:], modE[:, half * 512:(half + 1) * 512], M, ["pW%d" % half, "modB"], ["m3"])
                tt(x1t[s4][:, half * 512:(half + 1) * 512], m3[:], xt[sl][:, half * 512:(half + 1) * 512], A, ["m3", n_("xe", t)], [X1])
            dma(C.x1[t * 128:(t + 1) * 128, :], x1t[s4][:], [X1], ["x1"])
            act(junk[:], x1t[s4][:], AF.Square, [X1], ["junk", ST], accum_out=stat[s4][:, 0:1])

        def st3b(t):
            s4 = t % 4
            ST = "stat%d" % s4
            ts(stat[s4][:, 1:2], stat[s4][:, 0:1], 1.0 / D, RMS_EPS, M, A, [ST], [ST])
            act(stat[s4][:, 2:3], stat[s4][:, 1:2], AF.Sqrt, [ST], [ST])

        def st3c(t):
            s4 = t % 4
            X1, ST, H2 = "x1t%d" % s4, "stat%d" % s4, "h2_%d" % (t % 3)
            sc.op("dve", lambda e: e.reciprocal(out=stat[s4][:, 3:4], in_=stat[s4][:, 2:3]), [ST], [ST])
            stt(tmpn[:], x1t[s4][:], stat[s4][:, 3:4], G2[:], M, M, [X1, ST, "G2"], ["tmpn"])
            tt(h2[t % 3][:], tmpn[:], modE[:, D:2 * D], A, ["tmpn", "modB"], [H2])
            cp(h2hi[s4][:], h2[t % 3][:], [H2], ["h2hi%d" % s4], eng="pool")

        def st3d(t):
            s4 = t % 4
            H2 = "h2_%d" % (t % 3)
            tt(h2lo[t % 3][:], h2[t % 3][:], h2hi[s4][:], SUB, [H2, "h2hi%d" % s4], ["h2lo%d" % (t % 3)])

        def st4(t):
            s4, s3, s8 = t % 4, t % 3, t % 8
            for kc in range(8):
                tr(pTh[:, kc, :], h2hi[s4][:, kc * 128:(kc + 1) * 128], C.identB[:], ["h2hi%d" % s4, "identB"], ["pTh"])
            cp(h2Tb[s8][:], pTh[:], ["pTh"], ["h2Tb%d" % s8], eng="act")
            for kc in range(8):
                tr(pTl[:, kc, :], h2lo[s3][:, kc * 128:(kc + 1) * 128], C.identB[:], ["h2lo%d" % s3, "identB"], ["pTl"])
            cp(h2Tl[s8][:], pTl[:], ["pTl"], ["h2Tl%d" % s8])
            dma(C.h2T_d[:, t, :, :], h2Tb[s8][:], ["h2Tb%d" % s8], ["h2T_d"])

        def st5(t):
            if t % 4 != 3:
                return
            t0 = t - 3
            for i in range(4):
                s8 = (t0 + i) % 8
                k_ = 0
                for kc in range(8):
                    for a_, an_, b_ in ((h2Tb[s8], "h2Tb%d" % s8, wrh), (h2Tb[s8], "h2Tb%d" % s8, wrl), (h2Tl[s8], "h2Tl%d" % s8, wrh)):
                        mm(pL[:, i, :], a_[:, kc, :], b_[:, kc, :], k_ == 0, k_ == 23, [an_, "wrh"], ["pL"])
                        k_ += 1
            act(scs[:], pL[:], AF.Sigmoid, ["pL"], ["scs"])
            R = ["rt"]
            f2 = lambda ap: ap.rearrange("p a b -> p (a b)")
            v3 = lambda ap: ap.rearrange("p a (g k) -> p (a g) k", k=8)
            b8 = lambda ap: f2(ap).unsqueeze(2).to_broadcast([128, 32, 8])
            tt(selv[:], scs[:], rb[:].unsqueeze(1).to_broadcast([128, 4, 64]), A, ["scs", "rb"], R)
            sc.op("dve", lambda e: e.tensor_reduce(out=f2(mx1[:]), in_=v3(selv[:]), axis=AX.X, op=ALU.max), R, R)
            tt(v3(eq[:]), v3(selv[:]), b8(mx1[:]), ALU.is_equal, R, R)
            stt(f2(sel2[:]), f2(eq[:]), -1e9, f2(selv[:]), M, A, R, R)
            sc.op("dve", lambda e: e.tensor_reduce(out=f2(mx2[:]), in_=v3(sel2[:]), axis=AX.X, op=ALU.max), R, R)
            tt(gs[:], mx1[:], mx2[:], A, R, R)
            for i in range(4):
                sc.op("dve", lambda e, i=i: e.max(out=t8[:, i, :], in_=gs[:, i, :]), R, ["t8_%d" % i])
            tt(gmk[:], gs[:], t8[:, :, 3:4].to_broadcast([128, 4, 8]), ALU.is_ge, R + ["t8_%d" % i for i in range(4)], R)
            ts(f2(gng[:]), f2(gmk[:]), 1e9, -1e9, M, A, R, R)
            tt(v3(sel2[:]), v3(selv[:]), b8(gmk[:]), M, R, R)
            tt(v3(sel2[:]), v3(sel2[:]), b8(gng[:]), A, R, R)
            for i in range(4):
                sc.op("dve", lambda e, i=i: e.max(out=t8[:, i, :], in_=sel2[:, i, :]), R, ["t8_%d" % i])
            tt(eq[:], sel2[:], t8[:, :, 7:8].to_broadcast([128, 4, 64]), ALU.is_ge, R + ["t8_%d" % i for i in range(4)], R)
            tt(f2(eq[:]), f2(eq[:]), f2(scs[:]), M, R + ["scs"], R)
            sc.op("dve", lambda e: e.tensor_reduce(out=den[:, 0:4], in_=eq[:], axis=AX.X, op=ALU.add), R, R)
            sc.op("dve", lambda e: e.reciprocal(out=den[:, 4:8], in_=den[:, 0:4]), R, R)
            stt(C.gates[:, t0:t0 + 4, :], eq[:], 2.5, den[:, 4:8].unsqueeze(2).to_broadcast([128, 4, 64]), M, M, R, ["gates"])

        stages = [st0, st1, st2, st3, st3b, st3c, st3d, st4, st5]
        for step in range(NT + len(stages) - 1):
            for k in reversed(range(len(stages))):
                t = step - k
                if 0 <= t < NT:
                    stages[k](t)
        if C.dbg:
            gd = C.dscr("gates_d", [128, 32, 64])
            dma(gd, C.gates[:], ["gates"], ["gates_d"])
        sc.barrier()


def phase_moe(C):
    nc, sc = C.nc, C.sc
    H = mk_helpers(sc)
    tt, ts, stt, act, cp, ms, mm, tr, dma = H.tt, H.ts, H.stt, H.act, H.cp, H.ms, H.mm, H.tr, H.dma
    M, A = ALU.mult, ALU.add
    din = C.din
    d_wg = din("w_gate", [64, D, 256]); d_wu = din("w_up", [64, D, 256]); d_wd = din("w_down", [64, 256, D])
    d_sg = din("ws_gate", [D, 256]); d_su = din("ws_up", [D, 256]); d_sd = din("ws_down", [256, D])
    NE = int(C.n_experts)
    with ExitStack() as ph:
        sbp = lambda name, shape, dt=F32: ph.enter_context(nc.sbuf_tensor(uniq(name), list(shape), dt))
        psp = lambda name, shape, dt=F32: ph.enter_context(nc.psum_tensor(uniq(name), list(shape), dt))
        h2T = sbp("h2T", [128, 16, 8, 128], BF16)
        gf = sbp("gf", [128, D])
        dma(gf[:], C.mod_d[:, 5 * D:6 * D], ["mod_d"], ["modB"])
        acc = sbp("acc", [128, 16, D])
        wgs = sbp("wgs", [128, 8, 256]); wus = sbp("wus", [128, 8, 256]); wds = sbp("wds", [128, 2, D])
        wgb = [sbp("wgb%d" % i, [128, 8, 256], BF16) for i in range(2)]
        wub = [sbp("wub%d" % i, [128, 8, 256], BF16) for i in range(2)]
        wdb = [sbp("wdb%d" % i, [128, 2, D], BF16) for i in range(2)]
        sg = [sbp("sg%d" % i, [128, 512]) for i in range(2)]
        hid = [sbp("hid%d" % i, [128, 2, 512], BF16) for i in range(2)]
        x1t = [sbp("x1m%d" % i, [128, D]) for i in range(2)]
        ot = [sbp("ot%d" % i, [128, D]) for i in range(2)]
        pG = [psp("pG%d" % i, [128, 512]) for i in range(2)]
        pU = [psp("pU%d" % i, [128, 512]) for i in range(2)]
        pD = [psp("pD%d" % i, [128, 512]) for i in range(4)]
        dcount = [0]
        pend_back = [None]
        for half in range(2):
            dma(h2T[:], C.h2T_d[:, half * 16:(half + 1) * 16, :, :], ["h2T_d"], ["h2T"])
            ms(acc[:].rearrange("p a b -> p (a b)"), 0.0, ["acc"], eng="pool")
            for e in range(-1, NE):
                sl = (e + 1) % 2
                if e < 0:
                    srcs = (d_sg, d_su, d_sd)
                else:
                    srcs = (d_wg[e], d_wu[e], d_wd[e])
                dma(wgs[:], srcs[0].rearrange("(kc p) h -> p kc h", p=128), [], ["wgs"])
                dma(wus[:], srcs[1].rearrange("(kc p) h -> p kc h", p=128), [], ["wus"])
                dma(wds[:], srcs[2].rearrange("(hc p) n -> p hc n", p=128), [], ["wds"])
                cp(wgb[sl][:], wgs[:], ["wgs"], ["wgb%d" % sl], eng="pool")
                cp(wub[sl][:], wus[:], ["wus"], ["wub%d" % sl], eng="pool")
                cp(wdb[sl][:], wds[:], ["wds"], ["wdb%d" % sl], eng="pool")
                for tg in range(4):
                    def front_q(q, tg=tg, sl=sl):
                        hs_ = tg % 2
                        hc = q // 2
                        if q % 2 == 0:
                            for kc in range(8):
                                mm(pG[hc][:], wgb[sl][:, kc, hc * 128:(hc + 1) * 128], h2T[:, tg * 4:(tg + 1) * 4, kc, :],
                                   kc == 0, kc == 7, ["wgb%d" % sl, "h2T"], ["pG%d" % hc])
                            act(sg[hc][:], pG[hc][:], AF.Silu, ["pG%d" % hc], ["sg%d" % hc])
                        else:
                            for kc in range(8):
                                mm(pU[hc][:], wub[sl][:, kc, hc * 128:(hc + 1) * 128], h2T[:, tg * 4:(tg + 1) * 4, kc, :],
                                   kc == 0, kc == 7, ["wub%d" % sl, "h2T"], ["pU%d" % hc])
                            tt(hid[hs_][:, hc, :], sg[hc][:], pU[hc][:], M, ["sg%d" % hc, "pU%d" % hc], ["hid%d" % hs_])

                    def back_q(q, tg=tg, sl=sl, e=e):
                        hs_ = tg % 2
                        sub = q
                        tile_ = tg * 4 + sub
                        for nh in range(2):
                            pd = dcount[0] % 4
                            dcount[0] += 1
                            for hc in range(2):
                                mm(pD[pd][:], hid[hs_][:, hc, sub * 128:(sub + 1) * 128], wdb[sl][:, hc, nh * 512:(nh + 1) * 512],
                                   hc == 0, hc == 1, ["hid%d" % hs_, "wdb%d" % sl], ["pD%d" % pd])
                            gsc = 1.0 if e < 0 else C.gates[:, half * 16 + tile_, e:e + 1]
                            an = "acc%d_%d" % (tile_, nh)
                            stt(acc[:, tile_, nh * 512:(nh + 1) * 512], pD[pd][:], gsc, acc[:, tile_, nh * 512:(nh + 1) * 512],
                                M, A, ["pD%d" % pd, "acc", "gates"], [an])
                    prev = pend_back[0]
                    for q in range(4):
                        front_q(q)
                        if prev is not None:
                            prev(q)
                    pend_back[0] = back_q
            for q in range(4):
                pend_back[0](q)
            pend_back[0] = None
            for tl_ in range(16):
                t = half * 16 + tl_
                sl = tl_ % 2
                dma(x1t[sl][:], C.x1[t * 128:(t + 1) * 128, :], ["x1"], ["x1m%d" % sl])
                rd = ["acc"] + ["acc%d_%d" % (tl_, nh) for nh in range(2)]
                tt(ot[sl][:], acc[:, tl_, :], gf[:], M, rd + ["modB"], ["ot%d" % sl])
                tt(ot[sl][:], ot[sl][:], x1t[sl][:], A, ["ot%d" % sl, "x1m%d" % sl], ["ot%d" % sl], eng="pool")
                dma(C.out[t * 128:(t + 1) * 128, :], ot[sl][:], ["ot%d" % sl], ["out"])
            sc.barrier()


def phase_ab(C):
    nc, sc = C.nc, C.sc
    H = mk_helpers(sc)
    tt, ts, stt, act, cp, ms, mm, tr, dma = H.tt, H.ts, H.stt, H.act, H.cp, H.ms, H.mm, H.tr, H.dma
    M, A = ALU.mult, ALU.add
    x, w_in, g_mix, proj = C.x_in, C.w_in, C.g_mix, C.proj
    with ExitStack() as pa:
        sba = lambda name, shape, dt=F32: pa.enter_context(nc.sbuf_tensor(uniq(name), list(shape), dt))
        psa = lambda name, shape, dt=F32: pa.enter_context(nc.psum_tensor(uniq(name), list(shape), dt))
        Gm = sba("Gm", [128, D])
        modB = sba("modA", [128, 2 * D])
        dma(modB[:], C.mod_d[:, 0:2 * D], ["mod_d"], ["modB"])
        winb = sba("winb", [128, 8, INW], BF16)
        nch = [(0, 512), (512, 512), (1024, 512), (1536, 280), (1816, 512), (2328, 512), (2840, 512), (3352, 512)]
        with ExitStack() as pw:
            wst = [pw.enter_context(nc.sbuf_tensor(uniq("wst%d" % i), [128, 8, 512], F32)) for i in range(2)]
            dma(Gm[:], g_mix.partition_broadcast(128), [], ["Gm"])
            stt(Gm[:], modB[:, D:2 * D], 1.0, Gm[:], A, M, ["modB", "Gm"], ["Gm"])
            wiv = w_in.rearrange("(kc p) n -> p kc n", p=128)
            for ci, (n0, nw) in enumerate(nch):
                sl = ci % 2
                dma(wst[sl][:, :, 0:nw], wiv[:, :, n0:n0 + nw], [], ["wst%d" % sl])
                cp(winb[:, 0:4, n0:n0 + nw], wst[sl][:, 0:4, 0:nw], ["wst%d" % sl], ["winb"], eng="pool")
                cp(winb[:, 4:8, n0:n0 + nw], wst[sl][:, 4:8, 0:nw], ["wst%d" % sl], ["winb"])
            sc.barrier()
        NB = 3
        mk = lambda nm, shape, dt=F32, n=NB: [sba("%s%d" % (nm, i), shape, dt) for i in range(n)]
        xt = mk("xt", [128, D]); stat = mk("stat", [128, 4]); hb = mk("hb", [128, D], BF16)
        hT = mk("hT", [128, 8, 128], BF16)
        ob = mk("ob", [128, 1816], n=2); obg = mk("obg", [128, 2048], BF16, n=2)
        junk = sba("junk", [128, D], BF16); tmp = sba("tmpn", [128, D])
        pT = psa("pT", [128, 8, 128], BF16)
        pp = [psa("pp%d" % i, [128, 512]) for i in range(6)]
        n_ = lambda nm, t: "%s%d" % (nm, t % NB)
        pcnt = [0]

        def a0(t):
            dma(xt[t % NB][:], x[t * 128:(t + 1) * 128, :], [], [n_("xt", t)])

        def a1(t):
            sl = t % NB
            X, ST = n_("xt", t), n_("stat", t)
            act(junk[:], xt[sl][:], AF.Square, [X], ["junk", ST], accum_out=stat[sl][:, 0:1])
            ts(stat[sl][:, 1:2], stat[sl][:, 0:1], 1.0 / D, RMS_EPS, M, A, [ST], [ST])
            act(stat[sl][:, 2:3], stat[sl][:, 1:2], AF.Sqrt, [ST], [ST])

        def a2(t):
            sl = t % NB
            X, ST = n_("xt", t), n_("stat", t)
            sc.op("dve", lambda e: e.reciprocal(out=stat[sl][:, 3:4], in_=stat[sl][:, 2:3]), [ST], [ST])
            stt(tmp[:], xt[sl][:], stat[sl][:, 3:4], Gm[:], M, M, [X, ST, "Gm"], ["tmpn"])
            tt(hb[sl][:], tmp[:], modB[:, 0:D], A, ["tmpn", "modB"], [n_("hb", t)])

        def a3(t):
            sl = t % NB
            for kc in range(8):
                tr(pT[:, kc, :], hb[sl][:, kc * 128:(kc + 1) * 128], C.identB[:], [n_("hb", t), "identB"], ["pT"])
            cp(hT[sl][:], pT[:], ["pT"], [n_("hT", t)], eng="act")

        def a4(t):
            sl = t % NB
            s2 = t % 2
            OB, OG = "ob%d" % s2, "obg%d" % s2
            for ci, (n0, nw) in enumerate(nch):
                pb = pcnt[0] % 6
                pcnt[0] += 1
                for kc in range(8):
                    mm(pp[pb][:, 0:nw], hT[sl][:, kc, :], winb[:, kc, n0:n0 + nw], kc == 0, kc == 7, [n_("hT", t), "winb"], ["pp%d" % pb])
                if n0 >= 1816:
                    act(obg[s2][:, n0 - 1816:n0 - 1816 + nw], pp[pb][:, 0:nw], AF.Sigmoid, ["pp%d" % pb], [OG])
                else:
                    cp(ob[s2][:, n0:n0 + nw], pp[pb][:, 0:nw], ["pp%d" % pb], [OB])
            dma(proj[t * 128:(t + 1) * 128, :], ob[s2][:], [OB], ["proj"])
            dma(C.gms[t * 128:(t + 1) * 128, :], obg[s2][:], [OG], ["gms"])

        stages = [a0, a1, a2, a3, a4]
        for step in range(NT + len(stages) - 1):
            for k in reversed(range(len(stages))):
                t = step - k
                if 0 <= t < NT:
                    stages[k](t)
        sc.barrier()


def build(dbg=False, phases=None):
    if phases is None:
        phases = ALL_PHASES
    nc = bass.Bass("TRN2", target_bir_lowering=False)
    sc = Sched()
    C = Ctx()
    global LAST_SC
    LAST_SC = sc
    C.nc, C.sc, C.dbg, C.phases = nc, sc, dbg, phases

    def din(name, shape, dt=F32):
        return nc.dram_tensor(name, list(shape), dt, kind="ExternalInput").ap()

    def dscr(name, shape, dt=F32, producer=None):
        kind = "ExternalOutput" if dbg else "Internal"
        if dbg and producer is not None and producer not in phases:
            kind = "ExternalInput"
        return nc.dram_tensor(name, list(shape), dt, kind=kind).ap()
    C.din, C.dscr = din, dscr

    x = din("x", [S, D])
    cT = din("cT", [128, 8])
    w_ada = din("w_ada", [D, 6 * D])
    b_ada = din("b_ada", [1, 6 * D])
    g_mix = din("g_mix", [1, D])
    w_in = din("w_in", [D, INW])
    identf = din("identf", [128, 128])
    out = nc.dram_tensor("out", [S, D], F32, kind="ExternalOutput").ap()
    proj = dscr("proj", [S, 1816], producer="ab")
    C.gms = dscr("gms", [S, 2048], BF16, producer="ab")
    C.proj = proj
    C.yscr = dscr("yscr", [S, 512], producer="s5")
    C.s5T_d = dscr("s5T_d", [128, 32, 4, 128], BF16, producer="s5")
    C.nsaT_d = dscr("nsaT_d", [128, 32, 4, 128], BF16, producer="nsa")
    C.x1 = dscr("x1", [S, D], producer="e")
    C.h2T_d = dscr("h2T_d", [128, 32, 8, 128], BF16, producer="e")
    C.x_in = x
    C.mod_d = dscr("mod_d", [128, 6 * D], producer="0")
    C.w_in, C.g_mix = w_in, g_mix
    C.out = out
    import os
    C.n_experts = os.environ.get("N_EXPERTS", "64")

    es = ExitStack()
    with es:
        def sb(name, shape, dt=F32):
            return es.enter_context(nc.sbuf_tensor(uniq(name), list(shape), dt))

        def ps(name, shape, dt=F32):
            return es.enter_context(nc.psum_tensor(uniq(name), list(shape), dt))

        sems = {e: es.enter_context(nc.semaphore("s_" + e)) for e in ENGS}
        dsems = [es.enter_context(nc.semaphore("d%d" % i)) for i in range(N_DSEM)]

        identF = sb("identF", [128, 128])
        identB = sb("identB", [128, 128], BF16)
        ones = sb("ones", [128, 128])
        sc.dma(lambda e: e.dma_start(out=identF[:], in_=identf), writes=["identF"])
        sc.op("dve", lambda e: e.tensor_copy(out=identB[:], in_=identF[:]), ["identF"], ["identB"])
        sc.op("dve", lambda e: e.memset(ones[:], 1.0), [], ["ones"])

        C.identF, C.identB, C.ones = identF, identB, ones
        with ExitStack() as p0:
          if "0" in phases:
              def sb0(name, shape, dt=F32):
                  return p0.enter_context(nc.sbuf_tensor(uniq(name), list(shape), dt))
              modB = sb0("modB", [128, 6 * D])
              csb = sb0("csb", [128, 8])
              csl = sb0("csl", [128, 8])
              lhsc = sb0("lhsc", [128, 8, 128])
              bada = sb0("bada", [1, 6 * D])
              wbuf = [sb0("wada%d" % i, [128, 8, 512]) for i in range(2)]
              psm = [p0.enter_context(nc.psum_tensor(uniq("psm%d" % i), [128, 512], F32)) for i in range(2)]
              sc.dma(lambda e: e.dma_start(out=csb[:], in_=cT), writes=["csb"])
              sc.dma(lambda e: e.dma_start(out=bada[:], in_=b_ada), writes=["bada"])
              sc.op("act", lambda e: e.activation(out=csl[:], in_=csb[:], func=AF.Silu), ["csb"], ["csl"])
              for kc in range(8):
                  sc.op("dve", lambda e, kc=kc: e.tensor_scalar(
                      out=lhsc[:, kc, :], in0=ones[:], scalar1=csl[:, kc:kc + 1], scalar2=None,
                      op0=ALU.mult), ["csl", "ones"], ["lhsc"])
              wv = w_ada.rearrange("(kc p) n -> p kc n", p=128)
              for n in range(12):
                  sl = n % 2
                  sc.dma(lambda e, n=n, sl=sl: e.dma_start(out=wbuf[sl][:], in_=wv[:, :, n * 512:(n + 1) * 512]),
                         writes=["wada%d" % sl])
                  for kc in range(8):
                      sc.op("pe", lambda e, kc=kc, sl=sl: e.matmul(
                          psm[sl][:], lhsT=lhsc[:, kc, :], rhs=wbuf[sl][:, kc, :], start=(kc == 0), stop=False),
                          ["lhsc", "wada%d" % sl], ["psm%d" % sl])
                  sc.op("pe", lambda e, n=n, sl=sl: e.matmul(
                      psm[sl][:], lhsT=ones[0:1, :], rhs=bada[0:1, n * 512:(n + 1) * 512], start=False, stop=True),
                      ["ones", "bada"], ["psm%d" % sl])
                  sc.op("act", lambda e, n=n, sl=sl: e.activation(
                      out=modB[:, n * 512:(n + 1) * 512], in_=psm[sl][:], func=AF.Identity),
                      ["psm%d" % sl], ["modB"])
              sc.dma(lambda e: e.dma_start(out=C.mod_d, in_=modB[:]), reads=["modB"], writes=["mod_d"])
              sc.barrier()

        if "ab" in phases:
            phase_ab(C)
            sc.barrier()

        if "s5" in phases:
            phase_s5(C)
            sc.barrier()
        if "nsa" in phases:
            phase_nsa(C)
            sc.barrier()
        C.gates = sb("gates", [128, 32, 64])
        if "e" in phases:
            phase_e(C)
            sc.barrier()
        elif dbg and "moe" in phases:
            gin = din("gates_in", [128, 32, 64])
            sc.dma(lambda e: e.dma_start(out=C.gates[:], in_=gin), writes=["gates"])
            sc.barrier()
        if "moe" in phases:
            phase_moe(C)
            sc.barrier()

        sc.barrier()
        run = sc.emit(nc, sems, dsems)
        with nc.Block() as block:
            @block.tensor
            def _(e):
                run("pe", e)

            @block.scalar
            def _(e):
                run("act", e)

            @block.vector
            def _(e):
                run("dve", e)

            @block.gpsimd
            def _(e):
                run("pool", e)

            @block.sync
            def _(e):
                run("sp", e)
    return nc


def s5_inputs(inp):
    f = lambda a: np.ascontiguousarray(a, dtype=np.float32)
    pl = lambda a: f(a.reshape(16, 2, 64).transpose(1, 2, 0).reshape(128, 16))
    ldt = inp["s5_log_dt"][0].reshape(16, 2).T
    b4 = lambda a: f(a.reshape(16, 2, 64, 16).transpose(1, 2, 0, 3).reshape(128, 256))
    c4 = lambda a: f(a.reshape(16, 2, 16, 64).transpose(1, 3, 0, 2).reshape(128, 256))
    m = np.zeros((128, 4), np.float32)
    m[:64, 0] = 1.0; m[64:, 1] = 1.0; m[:64, 2] = -1.0; m[64:, 3] = -1.0
    return {
        "s5_lamr": pl(inp["s5_lambda_re"][0]), "s5_lami": pl(inp["s5_lambda_im"][0]),
        "s5_ldt": f(np.repeat(ldt[:, None, :], 64, axis=1).reshape(128, 16)),
        "s5_bre": b4(inp["s5_b_re"][0]), "s5_bim": b4(inp["s5_b_im"][0]),
        "s5_cre": c4(inp["s5_c_re"][0]), "s5_cim": c4(inp["s5_c_im"][0]),
        "s5_dcol": f(np.tile(inp["s5_d"][0].T, (8, 1))),
        "mask01": m,
        "s5_wglu": f(inp["s5_w_glu"][0]),
        "s5_bgl": f(inp["s5_b_glu"][0].reshape(4, 128).T),
    }


def make_inputs(inp, b):
    f = lambda a: np.ascontiguousarray(a, dtype=np.float32)
    return {
        "x": f(inp["x"][b]),
        "cT": f(inp["c"][b].reshape(8, 128).T),
        "w_ada": f(inp["w_ada"][0]),
        "b_ada": f(inp["b_ada"][0][None, :]),
        "g_mix": f(inp["norm_mix_gain"][0][None, :]),
        "w_in": f(inp["w_in"][0]),
        "identf": np.eye(128, dtype=np.float32),
        **s5_inputs(inp),
        **nsa_inputs(inp),
        "w_branch_a": f(inp["w_branch_a"][0]), "w_branch_b": f(inp["w_branch_b"][0]), "w_out": f(inp["w_out"][0]),
        "g_ffn": f(inp["norm_ffn_gain"][0][None, :]), "w_router": f(inp["w_router"][0]),
        "router_bias": f(inp["router_bias"][0][None, :]),
        "w_gate": f(inp["w_gate"][0]), "w_up": f(inp["w_up"][0]), "w_down": f(inp["w_down"][0]),
        "ws_gate": f(inp["ws_gate"][0]), "ws_up": f(inp["ws_up"][0]), "ws_down": f(inp["ws_down"][0]),
    }


def kernel(**inputs):
    nc = build(dbg=False)
    in_maps = [make_inputs(inputs, b) for b in range(8)]
    res = run_bass_kernel_spmd(nc, in_maps, core_ids=list(range(8)))
    return np.stack([r["out"] for r in res.results], axis=0).astype(np.float32)
```

```python
import numpy as np
from contextlib import ExitStack
import concourse.bass as bass
import concourse.mybir as mybir
from concourse.bass_utils import run_bass_kernel_spmd

F32 = mybir.dt.float32
BF16 = mybir.dt.bfloat16
AF = mybir.ActivationFunctionType
ALU = mybir.AluOpType
AX = mybir.AxisListType

S = 4096
D = 1024
NT = S // 128
INW = 3864
RMS_EPS = 1e-6
ENGS = ("pe", "act", "dve", "pool", "sp")
N_DSEM = 24
NOSAME = False


class Sched:
    def __init__(self):
        self.ops = {e: [] for e in ENGS}
        self.cnt = {e: 0 for e in ENGS}
        self.waited = {e: {} for e in ENGS}
        self.res = {}
        self.dcnt = [0] * N_DSEM
        self.dnext = 0

    def _collect(self, reads, writes):
        evs = {}

        def add(k, v):
            if v > evs.get(k, 0):
                evs[k] = v
        for r in reads:
            st = self.res.get(r)
            if st and st["w"]:
                add(*st["w"])
        for w in writes:
            st = self.res.get(w)
            if st:
                if st["w"]:
                    add(*st["w"])
                for k, v in st["r"].items():
                    add(k, v)
        return evs

    def _commit(self, ev, reads, writes):
        for r in reads:
            st = self.res.setdefault(r, {"w": None, "r": {}})
            if ev[1] > st["r"].get(ev[0], 0):
                st["r"][ev[0]] = ev[1]
        for w in writes:
            self.res[w] = {"w": ev, "r": {}}

    def _waits(self, eng, evs):
        out = []
        for k, v in evs.items():
            if isinstance(k, int):
                v = self.dcnt[k]
            elif k == eng and (eng == "pe" or NOSAME):
                continue
            if self.waited[eng].get(k, 0) >= v:
                continue
            self.waited[eng][k] = v
            out.append((k, v))
        return out

    def op(self, eng, fn, reads=(), writes=()):
        waits = self._waits(eng, self._collect(reads, writes))
        self.cnt[eng] += 1
        ev = (eng, self.cnt[eng])
        self.ops[eng].append((waits, fn, (eng, 1)))
        self._commit(ev, reads, writes)

    def dma(self, fn, reads=(), writes=(), q="sp"):
        waits = self._waits(q, self._collect(reads, writes))
        s = self.dnext
        self.dnext = (self.dnext + 1) % N_DSEM
        self.dcnt[s] += 16
        ev = (s, self.dcnt[s])
        self.ops[q].append((waits, fn, (s, 16)))
        self._commit(ev, reads, writes)

    def barrier(self):
        for e in ENGS:
            waits = []
            for k in ENGS:
                v = self.cnt[k]
                if v > self.waited[e].get(k, 0):
                    self.waited[e][k] = v
                    waits.append((k, v))
            for s in range(N_DSEM):
                v = self.dcnt[s]
                if v > self.waited[e].get(s, 0):
                    self.waited[e][s] = v
                    waits.append((s, v))
            if waits:
                self.ops[e].append((waits, None, None))
        self.res = {}

    def emit(self, nc, sems, dsems):
        def run(engname, eng):
            for waits, fn, inc in self.ops[engname]:
                for k, v in waits:
                    eng.wait_ge(dsems[k] if isinstance(k, int) else sems[k], v)
                if fn is None:
                    continue
                ins = fn(eng)
                k, n = inc
                ins.then_inc(dsems[k] if isinstance(k, int) else sems[k], n)
        return run


ALL_PHASES = ("0", "ab", "s5", "nsa", "e", "moe")


class Ctx:
    pass


_UNIQ = [0]


def uniq(name):
    _UNIQ[0] += 1
    return "%s_u%d" % (name, _UNIQ[0])


def mk_helpers(sc):
    H = Ctx()

    def tt(o, a, b, op, r, w, eng="dve"):
        sc.op(eng, lambda e: e.tensor_tensor(out=o, in0=a, in1=b, op=op), r, w)

    def ts(o, a, s1, s2, op0, op1, r, w, eng="dve"):
        if op1 is None:
            sc.op(eng, lambda e: e.tensor_scalar(out=o, in0=a, scalar1=s1, scalar2=None, op0=op0), r, w)
        else:
            sc.op(eng, lambda e: e.tensor_scalar(out=o, in0=a, scalar1=s1, scalar2=s2, op0=op0, op1=op1), r, w)

    def stt(o, a, s, b, op0, op1, r, w):
        sc.op("dve", lambda e: e.scalar_tensor_tensor(out=o, in0=a, scalar=s, in1=b, op0=op0, op1=op1), r, w)

    def act(o, a, func, r, w, **kw):
        sc.op("act", lambda e: e.activation(out=o, in_=a, func=func, **kw), r, w)

    def cp(o, a, r, w, eng="dve"):
        if eng == "act":
            sc.op("act", lambda e: e.activation(out=o, in_=a, func=AF.Identity), r, w)
        else:
            sc.op(eng, lambda e: e.tensor_copy(out=o, in_=a), r, w)

    def ms(o, v, w, eng="dve"):
        sc.op(eng, lambda e: e.memset(o, v), [], w)

    def mm(o, l, rr, st, sp, r, w):
        sc.op("pe", lambda e: e.matmul(o, lhsT=l, rhs=rr, start=st, stop=sp), r, w)

    def tr(o, a, ident, r, w):
        sc.op("pe", lambda e: e.transpose(out=o, in_=a, identity=ident), r, w)

    def dma(o, a, r, w, q="sp"):
        sc.dma(lambda e: e.dma_start(out=o, in_=a), reads=r, writes=w, q=q)
    H.tt, H.ts, H.stt, H.act, H.cp, H.ms, H.mm, H.tr, H.dma = tt, ts, stt, act, cp, ms, mm, tr, dma
    return H


def phase_s5(C):
    nc, sc = C.nc, C.sc
    H = mk_helpers(sc)
    tt, ts, stt, act, cp, ms, mm, tr, dma = H.tt, H.ts, H.stt, H.act, H.cp, H.ms, H.mm, H.tr, H.dma
    M, A, SUB = ALU.mult, ALU.add, ALU.subtract
    P = ["prm"]
    lamr_d = C.din("s5_lamr", [128, 16]); lami_d = C.din("s5_lami", [128, 16]); ldt_d = C.din("s5_ldt", [128, 16])
    bre_d = C.din("s5_bre", [128, 256]); bim_d = C.din("s5_bim", [128, 256])
    cre_d = C.din("s5_cre", [128, 256]); cim_d = C.din("s5_cim", [128, 256])
    dcol_d = C.din("s5_dcol", [128, 32]); mask_d = C.din("mask01", [128, 4])
    wglu_d = C.din("s5_wglu", [512, 512]); bgl_d = C.din("s5_bgl", [128, 4])
    with ExitStack() as ph:
        sbp = lambda name, shape, dt=F32: ph.enter_context(nc.sbuf_tensor(uniq(name), list(shape), dt))
        BsT = sbp("BsT", [128, 32, 2, 128], BF16)
        EEm = sbp("EEm", [128, 32, 2, 144], BF16)
        Tb = sbp("Tb", [128, 32, 128], BF16)
        CPr = sbp("CPr", [128, 16, 9]); CPi = sbp("CPi", [128, 16, 9]); CPin = sbp("CPin", [128, 16, 9])
        wglu = sbp("wglu", [128, 4, 512], BF16); bgl = sbp("bgl", [128, 4])
        with ExitStack() as pr:
            sbr = lambda name, shape, dt=F32: pr.enter_context(nc.sbuf_tensor(uniq(name), list(shape), dt))
            psr = lambda name, shape, dt=F32: pr.enter_context(nc.psum_tensor(uniq(name), list(shape), dt))
            T_ = {n: sbr("q_" + n, [128, 16]) for n in
                  "lamr lami ldt dt magl ang m16 s16 c16 pr pi t1 t2 t3 numr den rden cr ci".split()}
            halfpi = sbr("halfpi", [128, 1])
            mask = sbr("mask", [128, 4])
            dcol = sbr("dcol", [128, 32])
            Bre = sbr("Bre", [128, 16, 16]); Bim = sbr("Bim", [128, 16, 16])
            Cre = sbr("Cre", [128, 16, 16]); Cim = sbr("Cim", [128, 16, 16])
            bbr = sbr("bbr", [128, 16, 16]); bbi = sbr("bbi", [128, 16, 16])
            bbrb = sbr("bbrb", [128, 16, 16], BF16); bbib = sbr("bbib", [128, 16, 16], BF16)
            tA = sbr("tA", [128, 16, 16]); tB = sbr("tB", [128, 16, 16])
            PWr = sbr("PWr", [128, 16, 9]); PWi = sbr("PWi", [128, 16, 9])
            Wr = sbr("Wr", [128, 16, 128]); Wi = sbr("Wi", [128, 16, 128])
            EEr = sbr("EEr", [128, 16, 144]); EEi = sbr("EEi", [128, 16, 144])
            Kall = sbr("Kall", [16, 32, 128])
            T32 = sbr("T32", [128, 32, 128])
            wgst = sbr("wgst", [128, 4, 512])
            ptf = psr("ptf", [128, 4, 128])
            pk = psr("pk", [16, 4, 128])
            for nm, dd in (("lamr", lamr_d), ("lami", lami_d), ("ldt", ldt_d)):
                dma(T_[nm][:], dd, [], P)
            dma(Bre[:].rearrange("p a b -> p (a b)"), bre_d, [], P); dma(Bim[:].rearrange("p a b -> p (a b)"), bim_d, [], P)
            dma(Cre[:].rearrange("p a b -> p (a b)"), cre_d, [], P); dma(Cim[:].rearrange("p a b -> p (a b)"), cim_d, [], P)
            dma(dcol[:], dcol_d, [], P); dma(mask[:], mask_d, [], P); dma(bgl[:], bgl_d, [], ["bgl"])
            dma(wgst[:], wglu_d.rearrange("(kc p) n -> p kc n", p=128), [], ["wgst"])
            cp(wglu[:], wgst[:], ["wgst"], ["wglu"], eng="pool")
            ms(halfpi[:], float(np.pi / 2), P)
            g = lambda n: T_[n][:]
            act(g("dt"), g("ldt"), AF.Exp, P, P)
            tt(g("magl"), g("lamr"), g("dt"), M, P, P); tt(g("ang"), g("lami"), g("dt"), M, P, P)
            act(g("m16"), g("magl"), AF.Exp, P, P, scale=1.0 / 16)
            act(g("s16"), g("ang"), AF.Sin, P, P, scale=1.0 / 16)
            act(g("c16"), g("ang"), AF.Sin, P, P, scale=1.0 / 16, bias=halfpi[:])
            tt(g("pr"), g("m16"), g("c16"), M, P, P); tt(g("pi"), g("m16"), g("s16"), M, P, P)

            def csq(r, i):
                tt(g("t1"), r, r, M, P, P); tt(g("t2"), i, i, M, P, P)
                stt(g("t3"), r, 2.0, i, M, M, P, P)
                tt(r, g("t1"), g("t2"), SUB, P, P); cp(i, g("t3"), P, P)
            for _ in range(4):
                csq(g("pr"), g("pi"))
            ms(PWr[:, :, 0:1], 1.0, P); ms(PWi[:, :, 0:1], 0.0, P)
            cp(PWr[:, :, 1], g("pr"), P, P); cp(PWi[:, :, 1], g("pi"), P, P)
            for k in range(2, 9):
                tt(g("t1"), PWr[:, :, k - 1], g("pr"), M, P, P); tt(g("t2"), PWi[:, :, k - 1], g("pi"), M, P, P)
                tt(PWr[:, :, k], g("t1"), g("t2"), SUB, P, P)
                tt(g("t1"), PWr[:, :, k - 1], g("pi"), M, P, P); tt(g("t2"), PWi[:, :, k - 1], g("pr"), M, P, P)
                tt(PWi[:, :, k], g("t1"), g("t2"), A, P, P)
            cp(CPr[:, :, 0], PWr[:, :, 8], P, P); cp(CPi[:, :, 0], PWi[:, :, 8], P, P)
            for s_ in range(1, 9):
                cp(CPr[:, :, s_], CPr[:, :, s_ - 1], P, P); cp(CPi[:, :, s_], CPi[:, :, s_ - 1], P, P)
                csq(CPr[:, :, s_], CPi[:, :, s_])
            ts(CPin[:], CPi[:], -1.0, None, M, None, P, P)
            ts(g("numr"), g("pr"), -1.0, None, A, None, P, P)
            tt(g("t1"), g("lamr"), g("lamr"), M, P, P); tt(g("t2"), g("lami"), g("lami"), M, P, P)
            tt(g("den"), g("t1"), g("t2"), A, P, P)
            sc.op("dve", lambda e: e.reciprocal(out=g("rden"), in_=g("den")), P, P)
            tt(g("t1"), g("numr"), g("lamr"), M, P, P); tt(g("t2"), g("pi"), g("lami"), M, P, P)
            tt(g("t1"), g("t1"), g("t2"), A, P, P); tt(g("cr"), g("t1"), g("rden"), M, P, P)
            tt(g("t1"), g("pi"), g("lamr"), M, P, P); tt(g("t2"), g("numr"), g("lami"), M, P, P)
            tt(g("t1"), g("t1"), g("t2"), SUB, P, P); tt(g("ci"), g("t1"), g("rden"), M, P, P)
            bc = lambda ap: ap.to_broadcast([128, 16, 16])
            crb, cib = bc(T_["cr"][:].unsqueeze(2)), bc(T_["ci"][:].unsqueeze(2))
            tt(tA[:], Bre[:], crb, M, P, P); tt(tB[:], Bim[:], cib, M, P, P); tt(bbr[:], tA[:], tB[:], SUB, P, P)
            tt(tA[:], Bim[:], crb, M, P, P); tt(tB[:], Bre[:], cib, M, P, P); tt(bbi[:], tA[:], tB[:], A, P, P)
            cp(bbrb[:], bbr[:], P, P); cp(bbib[:], bbi[:], P, P)
            Wr4 = Wr[:].rearrange("p a (i q) -> p a i q", q=16); Wi4 = Wi[:].rearrange("p a (i q) -> p a i q", q=16)
            for i in range(8):
                pwr, pwi = bc(PWr[:, :, 7 - i:8 - i]), bc(PWi[:, :, 7 - i:8 - i])
                tt(tA[:], bbr[:], pwr, M, P, P); tt(tB[:], bbi[:], pwi, M, P, P); tt(Wr4[:, :, i, :], tA[:], tB[:], SUB, P, P)
                tt(tA[:], bbr[:], pwi, M, P, P); tt(tB[:], bbi[:], pwr, M, P, P); tt(Wi4[:, :, i, :], tA[:], tB[:], A, P, P)
            EEr4 = EEr[:].rearrange("p a (k q) -> p a k q", q=16); EEi4 = EEi[:].rearrange("p a (k q) -> p a k q", q=16)
            for k in range(9):
                pwr, pwi = bc(PWr[:, :, k:k + 1]), bc(PWi[:, :, k:k + 1])
                tt(tA[:], Cre[:], pwr, M, P, P); tt(tB[:], Cim[:], pwi, M, P, P); tt(EEr4[:, :, k, :], tA[:], tB[:], SUB, P, P)
                tt(tA[:], Cre[:], pwi, M, P, P); tt(tB[:], Cim[:], pwr, M, P, P); tt(EEi4[:, :, k, :], tA[:], tB[:], A, P, P)
            EEm5 = EEm[:].rearrange("p (a b) c d -> p a b c d", b=2)
            for g2 in range(2):
                ts(EEm5[:, :, g2, 0, :], EEr[:], mask[:, g2:g2 + 1], None, M, None, P, P)
                ts(EEm5[:, :, g2, 1, :], EEi[:], mask[:, 2 + g2:3 + g2], None, M, None, P, P)
            ms(BsT[:].rearrange("p a b c -> p (a b c)"), 0.0, P, eng="pool")
            for p_ in range(16):
                for plane, W in ((0, Wr), (1, Wi)):
                    slot = (2 * p_ + plane) % 4
                    tr(ptf[:, slot, :], W[:, p_, :], C.identF[:], P + ["identF"], ["ptf%d" % slot])
                    cp(BsT[:, 2 * p_, plane, 0:64], ptf[:, slot, 0:64], ["ptf%d" % slot], P, eng="act")
                    cp(BsT[:, 2 * p_ + 1, plane, 64:128], ptf[:, slot, 64:128], ["ptf%d" % slot], P)
            for g_ in range(32):
                p_ = g_ // 2
                mm(pk[:, g_ % 4, :], bbrb[:, p_, :], EEm[:, g_, 0, 0:128], True, False, P, ["pk"])
                mm(pk[:, g_ % 4, :], bbib[:, p_, :], EEm[:, g_, 1, 0:128], False, True, P, ["pk"])
                if g_ % 4 == 3:
                    cp(Kall[:, g_ - 3:g_ + 1, :], pk[:], ["pk"], P, eng="act")
            ms(T32[:].rearrange("p a b -> p (a b)"), 0.0, P, eng="pool")
            for i in range(8):
                dma(T32[16 * i:16 * i + 16, :, 16 * i:128], Kall[0:16, :, 0:128 - 16 * i], P, P)
            for g_ in range(32):
                stt(Tb[:, g_, :], C.identF[:], dcol[:, g_:g_ + 1], T32[:, g_, :], M, A, P + ["identF"], P)
            sc.barrier()
        with ExitStack() as pm:
            sbm = lambda name, shape, dt=F32: pm.enter_context(nc.sbuf_tensor(uniq(name), list(shape), dt))
            psm_ = lambda name, shape, dt=F32: pm.enter_context(nc.psum_tensor(uniq(name), list(shape), dt))
            uch = [sbm("uch%d" % i, [128, 8, 128]) for i in range(2)]
            uchb = [sbm("uchb%d" % i, [128, 8, 128], BF16) for i in range(2)]
            UT = sbm("UT", [128, 8, 512], BF16)
            XA = [sbm("XAr", [128, 4, 513]), sbm("XAi", [128, 4, 513])]
            XB = [sbm("XBr", [128, 4, 513]), sbm("XBi", [128, 4, 513])]
            Xbf = [sbm("Xbfr", [128, 4, 513], BF16), sbm("Xbfi", [128, 4, 513], BF16)]
            ysb = [sbm("ysb%d" % i, [128, 8, 128]) for i in range(2)]
            ptu = psm_("ptu", [128, 8, 128], BF16)
            psS = [psm_("psS%d" % i, [128, 512]) for i in range(2)]
            psY = [psm_("psY%d" % i, [128, 4, 128]) for i in range(2)]
            for pl in range(2):
                ms(XA[pl][:, :, 0:1], 0.0, ["XA"]); ms(XB[pl][:, :, 0:1], 0.0, ["XB"])
            projv = C.proj.rearrange("(c j) n -> c j n", j=8)
            yv = C.yscr.rearrange("(c j) n -> c j n", j=8)
            for b in range(4):
                for ct in range(4):
                    sl = ct % 2
                    dma(uch[sl][:], projv[ct * 128:(ct + 1) * 128, :, b * 128:(b + 1) * 128], [], ["uch%d" % sl])
                    cp(uchb[sl][:].rearrange("c g (j q) -> c j g q", q=16),
                       uch[sl][:].rearrange("c j (g q) -> c j g q", q=16), ["uch%d" % sl], ["uchb%d" % sl], eng="pool")
                    for gl in range(8):
                        tr(ptu[:, gl, :], uchb[sl][:, gl, :], C.identB[:],
                           ["uchb%d" % sl, "identB"], ["ptu"])
                    cp(UT[:, :, ct * 128:(ct + 1) * 128], ptu[:], ["ptu"], ["UT"], eng="act")
                for pl_ in range(4):
                    g0 = 8 * b + 2 * pl_
                    for plane in range(2):
                        s_ = (2 * pl_ + plane) % 2
                        mm(psS[s_][:], BsT[:, g0, plane, :], UT[:, 2 * pl_, :], True, False, ["UT"], ["psS%d" % s_])
                        mm(psS[s_][:], BsT[:, g0 + 1, plane, :], UT[:, 2 * pl_ + 1, :], False, True, ["UT"], ["psS%d" % s_])
                        cp(XA[plane][:, pl_, 1:513], psS[s_][:], ["psS%d" % s_], ["XA"], eng=("act" if plane else "dve"))
                src, dst, sn, dn = XA, XB, "XA", "XB"
                for s_ in range(9):
                    sh = 1 << s_
                    for pl_ in range(4):
                        p_ = 4 * b + pl_
                        pr_, pi_, pin_ = CPr[:, p_, s_:s_ + 1], CPi[:, p_, s_:s_ + 1], CPin[:, p_, s_:s_ + 1]
                        lo = lambda t: t[:, pl_, 1:513 - sh]
                        hi = lambda t: t[:, pl_, 1 + sh:513]
                        stt(hi(dst[0]), lo(src[0]), pr_, hi(src[0]), M, A, [sn], [dn + "r%d" % pl_])
                        stt(hi(dst[1]), lo(src[1]), pr_, hi(src[1]), M, A, [sn], [dn + "i%d" % pl_])
                    for pl_ in range(4):
                        p_ = 4 * b + pl_
                        pr_, pi_, pin_ = CPr[:, p_, s_:s_ + 1], CPi[:, p_, s_:s_ + 1], CPin[:, p_, s_:s_ + 1]
                        lo = lambda t: t[:, pl_, 1:513 - sh]
                        hi = lambda t: t[:, pl_, 1 + sh:513]
                        stt(hi(dst[0]), lo(src[1]), pin_, hi(dst[0]), M, A, [sn, dn + "r%d" % pl_], [dn + "r%d" % pl_, dn])
                        stt(hi(dst[1]), lo(src[0]), pi_, hi(dst[1]), M, A, [sn, dn + "i%d" % pl_], [dn + "i%d" % pl_, dn])
                    cp(dst[0][:, :, 1:1 + sh], src[0][:, :, 1:1 + sh], [sn], [dn], eng="pool")
                    cp(dst[1][:, :, 1:1 + sh], src[1][:, :, 1:1 + sh], [sn], [dn], eng="pool")
                    src, dst, sn, dn = dst, src, dn, sn
                cp(Xbf[0][:], src[0][:], [sn], ["Xbf"], eng="act")
                cp(Xbf[1][:], src[1][:], [sn], ["Xbf"], eng="act")
                for ct in range(4):
                    sl = ct % 2
                    for half in range(2):
                        for glh in range(4):
                            gl = half * 4 + glh
                            g_ = 8 * b + gl
                            pl_ = gl // 2
                            o = psY[half][:, glh, :]
                            mm(o, UT[:, gl, ct * 128:(ct + 1) * 128], Tb[:, g_, :], True, False, ["UT"], ["psY%d" % half])
                            mm(o, Xbf[0][:, pl_, ct * 128:ct * 128 + 128], EEm[:, g_, 0, 16:144], False, False, ["Xbf"], ["psY%d" % half])
                            mm(o, Xbf[1][:, pl_, ct * 128:ct * 128 + 128], EEm[:, g_, 1, 16:144], False, True, ["Xbf"], ["psY%d" % half])
                        cp(ysb[sl][:, :, half * 64:(half + 1) * 64].rearrange("c j (g p) -> c j g p", p=16),
                           psY[half][:].rearrange("c g (j p) -> c j g p", p=16),
                           ["psY%d" % half], ["ysb%d" % sl], eng=("act" if half else "dve"))
                    dma(yv[ct * 128:(ct + 1) * 128, :, b * 128:(b + 1) * 128], ysb[sl][:], ["ysb%d" % sl], ["yscr"])
            sc.barrier()
        with ExitStack() as pq:
            sbq = lambda name, shape, dt=F32: pq.enter_context(nc.sbuf_tensor(uniq(name), list(shape), dt))
            psq = lambda name, shape, dt=F32: pq.enter_context(nc.psum_tensor(uniq(name), list(shape), dt))
            yt = [sbq("yt%d" % i, [128, 512]) for i in range(4)]
            g1 = [sbq("g1_%d" % i, [128, 512]) for i in range(2)]; g2t = [sbq("g2t%d" % i, [128, 512]) for i in range(2)]
            zb = [sbq("zb%d" % i, [128, 512], BF16) for i in range(2)]
            zT = [sbq("zT%d" % i, [128, 4, 512], BF16) for i in range(2)]
            sgt = [sbq("sgt%d" % i, [128, 512], BF16) for i in range(2)]
            ptz = psq("ptz", [128, 4, 128], BF16)
            psG = [psq("psG%d" % i, [128, 512]) for i in range(2)]
            s5T = sbq("s5T", [128, 32, 4, 128], BF16)
            KG = float(2.0 * np.sqrt(2.0 / np.pi))

            def q0(t):
                dma(yt[t % 4][:], C.yscr[t * 128:(t + 1) * 128, :], ["yscr"], ["yt%d" % (t % 4)])

            def q1(t):
                y_, Y, G1 = yt[t % 4], "yt%d" % (t % 4), "g1_%d" % (t % 2)
                g_ = g1[t % 2]
                tt(g_[:], y_[:], y_[:], M, [Y], [G1], eng="pool")
                ts(g_[:], g_[:], 0.044715, 1.0, M, A, [G1], [G1])
                tt(g_[:], g_[:], y_[:], M, [G1, Y], [G1])

            def q2(t):
                act(g2t[t % 2][:], g1[t % 2][:], AF.Sigmoid, ["g1_%d" % (t % 2)], ["g2t%d" % (t % 2)], scale=KG)

            def q3(t):
                sl = t % 2
                tt(zb[sl][:], g2t[sl][:], yt[t % 4][:], M, ["g2t%d" % sl, "yt%d" % (t % 4)], ["zb%d" % sl])

            def q4(t):
                sl = t % 2
                zs = (t // 4) % 2
                for kc in range(4):
                    tr(ptz[:, kc, :], zb[sl][:, kc * 128:(kc + 1) * 128], C.identB[:], ["zb%d" % sl, "identB"], ["ptz"])
                cp(zT[zs][:, :, (t % 4) * 128:(t % 4 + 1) * 128], ptz[:], ["ptz"], ["zT%d" % zs], eng="act")

            def q5(t):
                if t % 4 != 3:
                    return
                zs = (t // 4) % 2
                tg = t // 4
                for ncx in range(4):
                    s_ = ncx % 2
                    for kc in range(4):
                        mm(psG[s_][:], wglu[:, kc, ncx * 128:(ncx + 1) * 128], zT[zs][:, kc, :], kc == 0, kc == 3,
                           ["wglu", "zT%d" % zs], ["psG%d" % s_])
                    act(sgt[s_][:], psG[s_][:], AF.Sigmoid, ["psG%d" % s_, "bgl"], ["sgt%d" % s_], bias=bgl[:, ncx:ncx + 1])
                    tt(s5T[:, tg * 4:(tg + 1) * 4, ncx, :], zT[zs][:, ncx, :].rearrange("p (t j) -> p t j", j=128),
                       sgt[s_][:].rearrange("p (t j) -> p t j", j=128), M,
                       ["zT%d" % zs, "sgt%d" % s_], ["s5T"])

            qst = [q0, q1, q2, q3, q4, q5]
            for step in range(NT + len(qst) - 1):
                for k in reversed(range(len(qst))):
                    t = step - k
                    if 0 <= t < NT:
                        qst[k](t)
            dma(C.s5T_d, s5T[:], ["s5T"], ["s5T_d"])
            sc.barrier()


SLOPES = [2.0 ** (-(h + 1)) for h in range(8)]
NEGV = -30000.0


def nsa_consts():
    import ml_dtypes
    bf = ml_dtypes.bfloat16
    kl = np.arange(128)[:, None].astype(np.float64)
    tl = np.arange(512)[None, :].astype(np.float64)
    diffS = (tl - kl).astype(np.float32)
    diffC = (tl - 16 * kl - 31).astype(np.float32)
    negm = np.zeros((128, 19, 512), np.float32)
    for i in range(5):
        negm[:, i, :] = np.where(diffC + 512 * i >= 0, 0.0, NEGV)
    for m in range(4):
        negm[:, 5 + m, :] = np.where(diffS - 128 * m >= 0, 0.0, NEGV)
    for i, dl in enumerate((2, 1, 0, -1, -2, -3)):
        dd = diffS + 128 * dl
        negm[:, 9 + i, :] = np.where((dd >= 0) & (dd < 256), 0.0, NEGV)
    for i in range(4):
        negm[:, 15 + i, :] = negm[:, i, :]
        negm[127, 15 + i, :] = NEGV
    t = (np.arange(32)[None, :, None] * 128 + np.arange(128)[:, None, None])
    cur = t // 64
    j = np.arange(64)[None, None, :]
    valid = j <= cur
    forced = (j == 0) | (j == cur) | (j == cur - 1)
    mvadd = np.zeros((128, 32, 2, 64), np.float32)
    mvadd[:, :, 0, :] = valid
    mvadd[:, :, 1, :] = np.where(valid, forced * 1000.0, -1.0)
    ebc = np.zeros((128, 32, 128), np.float32)
    for kc in range(32):
        ebc[2 * kc, kc, :64] = 1.0
        ebc[2 * kc + 1, kc, 64:] = 1.0
    cmp_start = np.arange(255) * 16
    sel_start = np.arange(64) * 64
    ov = ((cmp_start[:, None] <= sel_start[None, :] + 63) & (cmp_start[:, None] + 31 >= sel_start[None, :]))
    ovp = np.zeros((256, 64), np.float32); ovp[:255] = ov
    blk = np.zeros((128, 128), np.float32); blk[:64, :64] = 1.0; blk[64:, 64:] = 1.0
    tlv = np.arange(512)
    auxl = np.zeros((128, 128), np.float32)
    auxl[[0, 1], :] = 1.0
    auxr = np.zeros((128, 8, 512), np.float32)
    for h in range(8):
        for base in (0,):
            auxr[base, h, :] = -SLOPES[h] * 16.0 * (tlv // 16)
            auxr[base + 1, h, :] = -SLOPES[h] * (tlv % 16)
    klv = np.arange(128).astype(np.float64)
    biasS = np.zeros((128, 8, 35), np.float32)
    biasC = np.zeros((128, 8, 8, 2), np.float32)
    for h in range(8):
        for di in range(35):
            biasS[:, h, di] = SLOPES[h] * (klv - 128.0 * (di - 3))
        for Q in range(8):
            for ct in range(2):
                biasC[:, h, Q, ct] = SLOPES[h] * (16.0 * klv + 31.0 - (512.0 * Q - 2048.0 * ct))
    return {
        "n_auxl": auxl.astype(bf), "n_auxr": auxr.astype(bf), "n_biasS": biasS, "n_biasC": biasC,
        "n_diffS": diffS, "n_diffC": diffC, "n_negm": negm.astype(bf), "n_mvadd": mvadd.astype(bf),
        "n_ebc": ebc.astype(bf), "n_ovl": np.ascontiguousarray(ovp.reshape(2, 128, 64).transpose(1, 0, 2)),
        "n_blk64": blk,
    }


def nsa_inputs(inp):
    f = lambda a: np.ascontiguousarray(a, dtype=np.float32)
    pe = inp["cmp_pe"][0]
    return {
        "n_qg": f(np.tile(inp["q_norm_gain"][0], 8)[None, :]),
        "n_kg1": f(np.tile(inp["k_norm_gain"][0, 1], 2)[None, :]),
        "n_kg2": f(np.tile(inp["k_norm_gain"][0, 2], 2)[None, :]),
        "n_kg0": f(np.tile(inp["k_norm_gain"][0, 0], 2)[:, None]),
        "n_peT": f(np.stack([pe[j].reshape(16, 2, 64).transpose(1, 2, 0).reshape(128, 16) for j in range(2)], axis=1)),
        "n_b1T": f(inp["cmp_b1"][0].reshape(2, 2, 128).transpose(2, 0, 1)),
        "n_w1": f(inp["cmp_w1"][0]),
        "n_w2": f(inp["cmp_w2"][0]),
        "n_b2k": f(np.tile(inp["cmp_b2"][0, 0], 2)[:, None]),
        "n_b2v": f(inp["cmp_b2"][0, 1][None, :]),
        "n_mask01": np.concatenate([np.repeat([[1.0, 0.0]], 64, 0), np.repeat([[0.0, 1.0]], 64, 0)]).astype(np.float32),
        **nsa_consts(),
    }


def phase_nsa(C):
    nc, sc = C.nc, C.sc
    H = mk_helpers(sc)
    tt, ts, stt, act, cp, ms, mm, tr, dma = H.tt, H.ts, H.stt, H.act, H.cp, H.ms, H.mm, H.tr, H.dma
    M, A, SUB = ALU.mult, ALU.add, ALU.subtract
    din = C.din
    d_qg = din("n_qg", [1, 512]); d_kg1 = din("n_kg1", [1, 128]); d_kg2 = din("n_kg2", [1, 128]); d_kg0 = din("n_kg0", [128, 1])
    d_peT = din("n_peT", [128, 2, 16]); d_b1T = din("n_b1T", [128, 2, 2]); d_w1 = din("n_w1", [2, 2048, 256])
    d_m01 = din("n_mask01", [128, 2]); d_w2 = din("n_w2", [2, 256, 64]); d_b2k = din("n_b2k", [128, 1]); d_b2v = din("n_b2v", [1, 64])
    d_auxl = din("n_auxl", [128, 128], BF16); d_auxr = din("n_auxr", [128, 8, 512], BF16)
    d_biasS = din("n_biasS", [128, 8, 35]); d_biasC = din("n_biasC", [128, 8, 8, 2])
    d_negm = din("n_negm", [128, 19, 512], BF16); d_mvadd = din("n_mvadd", [128, 32, 2, 64], BF16)
    d_ebc = din("n_ebc", [128, 32, 128], BF16); d_ovl = din("n_ovl", [128, 2, 64]); d_blk = din("n_blk64", [128, 128])
    proj = C.proj
    KG = float(2.0 * np.sqrt(2.0 / np.pi))
    with ExitStack() as ph:
        sbp = lambda name, shape, dt=F32: ph.enter_context(nc.sbuf_tensor(uniq(name), list(shape), dt))
        qT = sbp("qT", [128, 4, S], BF16)
        ksT2 = sbp("ksZ", [128, 2, 2, S], BF16)
        kwT2 = sbp("kwZ", [128, 2, 2, S], BF16)
        vs_aug = sbp("vs_aug", [128, 32, 2, 65], BF16)
        vw_aug = sbp("vw_aug", [128, 32, 2, 65], BF16)
        gn = sbp("gn", [128, 32, 24])
        kcT2 = sbp("kcZ", [128, 2, 2, 256], BF16)
        vcA = sbp("vcA", [128, 2, 2, 65], BF16)
        ovl = sbp("ovl", [128, 2, 64], BF16)
        with ExitStack() as p1:
            sb1 = lambda name, shape, dt=F32: p1.enter_context(nc.sbuf_tensor(uniq(name), list(shape), dt))
            ps1 = lambda name, shape, dt=F32: p1.enter_context(nc.psum_tensor(uniq(name), list(shape), dt))
            qg = sb1("qg", [128, 512]); kg1 = sb1("kg1", [128, 128]); kg2 = sb1("kg2", [128, 128])
            kg0 = sb1("kg0", [128, 1]); b2k = sb1("b2k", [128, 1]); b2v = sb1("b2v", [1, 64]); b2vb = sb1("b2vb", [1, 64], BF16)
            onesb = sb1("onesb", [1, 128], BF16)
            blk = sb1("blk", [128, 128])
            ovf = sb1("ovf", [128, 2, 64])
            pt = [sb1("pt%d" % i, [128, 1304]) for i in range(4)]
            sqs = [sb1("sq%d" % i, [128, 768]) for i in range(2)]
            st12s = [sb1("st12_%d" % i, [128, 12]) for i in range(3)]; st12bs = [sb1("st12b%d" % i, [128, 12]) for i in range(3)]
            qn = sb1("qn", [128, 512]); qnbs = [sb1("qnb%d" % i, [128, 512], BF16) for i in range(2)]
            kn = sb1("kn", [128, 256]); knbs = [sb1("knb%d" % i, [128, 2, 2, 2, 128], BF16) for i in range(2)]
            ptk = ps1("ptk", [128, 8, 128], BF16)
            ptr = ps1("ptr", [128, 8, 128], BF16)
            dma(qg[:], d_qg.partition_broadcast(128), [], ["qg"]); dma(kg1[:], d_kg1.partition_broadcast(128), [], ["kg"])
            dma(kg2[:], d_kg2.partition_broadcast(128), [], ["kg"]); dma(kg0[:], d_kg0, [], ["kg0"])
            dma(b2k[:], d_b2k, [], ["b2k"]); dma(b2v[:], d_b2v, [], ["b2v"]); dma(blk[:], d_blk, [], ["blk"])
            dma(ovf[:], d_ovl, [], ["ovf"])
            cp(ovl[:], ovf[:], ["ovf"], ["ovl"])
            cp(b2vb[:], b2v[:], ["b2v"], ["b2vb"])
            ms(onesb[:], 1.0, ["onesb"])
            ts(qg[:], qg[:], 0.125, None, M, None, ["qg"], ["qg"])
            ms(vs_aug[:, :, :, 64:65], 1.0, ["vs_aug"], eng="pool"); ms(vw_aug[:, :, :, 64:65], 1.0, ["vw_aug"], eng="pool")
            for i_ in range(2):
                ms(knbs[i_][:].rearrange("p a b c d -> p (a b c d)"), 0.0, ["knb%d" % i_])
            v64 = lambda ap: ap.rearrange("p (a d) -> p a d", d=64)

            def d0(t):
                dma(pt[t % 4][:], proj[t * 128:(t + 1) * 128, 512:1816], [], ["pt%d" % (t % 4)])

            def d1(t):
                p_, PT, s3 = pt[t % 4], "pt%d" % (t % 4), t % 3
                SQ, ST = "sq%d" % (t % 2), "st12_%d" % s3
                sq_ = sqs[t % 2]
                tt(sq_[:, 0:512], p_[:, 0:512], p_[:, 0:512], M, [PT], [SQ], eng="pool")
                tt(sq_[:, 512:640], p_[:, 768:896], p_[:, 768:896], M, [PT], [SQ], eng="pool")
                tt(sq_[:, 640:768], p_[:, 1024:1152], p_[:, 1024:1152], M, [PT], [SQ], eng="pool")
                cp(vs_aug[:, t, :, 0:64], v64(p_[:, 896:1024]), [PT], ["vs_aug"], eng="pool")
                cp(vw_aug[:, t, :, 0:64], v64(p_[:, 1152:1280]), [PT], ["vw_aug"], eng="pool")
                act(gn[:, t, :], p_[:, 1280:1304], AF.Sigmoid, [PT], ["gn"])

            def d2(t):
                s3 = t % 3
                SQ, ST = "sq%d" % (t % 2), "st12_%d" % s3
                sc.op("dve", lambda e: e.tensor_reduce(out=st12s[s3][:], in_=v64(sqs[t % 2][:]), axis=AX.X, op=ALU.add), [SQ], [ST])
                ts(st12s[s3][:], st12s[s3][:], 1.0 / 64, RMS_EPS, M, A, [ST], [ST])
                act(st12bs[s3][:], st12s[s3][:], AF.Sqrt, [ST], [ST + "b"])

            def d3(t):
                p_, PT, s3, s2 = pt[t % 4], "pt%d" % (t % 4), t % 3, t % 2
                ST = "st12_%d" % s3
                st_ = st12s[s3]
                sc.op("dve", lambda e: e.reciprocal(out=st_[:], in_=st12bs[s3][:]), [ST + "b"], [ST])
                tt(v64(qn[:]), v64(p_[:, 0:512]), st_[:, 0:8].unsqueeze(2).to_broadcast([128, 8, 64]), M, [PT, ST], ["qn"])
                tt(v64(kn[:, 0:128]), v64(p_[:, 768:896]), st_[:, 8:10].unsqueeze(2).to_broadcast([128, 2, 64]), M, [PT, ST], ["kn"])
                tt(v64(kn[:, 128:256]), v64(p_[:, 1024:1152]), st_[:, 10:12].unsqueeze(2).to_broadcast([128, 2, 64]), M, [PT, ST], ["kn"])
                tt(qnbs[s2][:], qn[:], qg[:], M, ["qn", "qg"], ["qnb%d" % s2])
                for w_, kg in ((0, kg1), (1, kg2)):
                    for par in range(2):
                        tt(knbs[s2][:, w_, :, par, par * 64:(par + 1) * 64], v64(kn[:, w_ * 128:(w_ + 1) * 128]), v64(kg[:]), M,
                           ["kn", "kg"], ["knb%d" % s2], eng=("pool" if par else "dve"))

            def d4(t):
                s2 = t % 2
                for c4 in range(4):
                    tr(ptr[:, c4, :], qnbs[s2][:, c4 * 128:(c4 + 1) * 128], C.identB[:], ["qnb%d" % s2, "identB"], ["ptr"])
                for w_ in range(2):
                    for hk in range(2):
                        for par in range(2):
                            tr(ptk[:, 4 * w_ + 2 * hk + par, :], knbs[s2][:, w_, hk, par, :], C.identB[:],
                               ["knb%d" % s2, "identB"], ["ptk"])
                cp(qT[:, :, t * 128:(t + 1) * 128], ptr[:, 0:4, :], ["ptr"], ["qT"], eng="act")
                cp(ksT2[:, :, :, t * 128:(t + 1) * 128], ptk[:, 0:4, :].rearrange("p (a b) j -> p a b j", b=2), ["ptk"], ["ksT2"], eng="act")
                cp(kwT2[:, :, :, t * 128:(t + 1) * 128], ptk[:, 4:8, :].rearrange("p (a b) j -> p a b j", b=2), ["ptk"], ["kwT2"], eng="act")

            dst = [d0, d1, d2, d3, d4]
            for step in range(NT + len(dst) - 1):
                for k in reversed(range(len(dst))):
                    t = step - k
                    if 0 <= t < NT:
                        dst[k](t)
            sc.barrier()
        import os
        NSTOP = 3
        if NSTOP < 2:
            return
        with ExitStack() as p2:
            sb2 = lambda name, shape, dt=F32: p2.enter_context(nc.sbuf_tensor(uniq(name), list(shape), dt))
            ps2 = lambda name, shape, dt=F32: p2.enter_context(nc.psum_tensor(uniq(name), list(shape), dt))
            PS = sb2("PS", [128, 4, 2048], BF16)
            pc = [sb2("pc%d" % i, [128, 2, 256]) for i in range(2)]
            pcb = [sb2("pcb%d" % i, [128, 4, 2, 64], BF16) for i in range(2)]
            w1st = sb2("w1st", [128, 16, 256])
            w1b = sb2("w1b", [128, 2, 16, 256], BF16)
            w2f = sb2("w2f", [128, 2, 2, 64]); w2kd = sb2("w2kd", [128, 2, 2, 64], BF16); w2v = sb2("w2v", [128, 2, 64], BF16)
            peT = sb2("peT", [128, 2, 16]); peTb = sb2("peTb", [128, 2, 16], BF16)
            b1T = sb2("b1T", [128, 2, 2]); bias1 = sb2("bias1", [128, 2, 2])
            m01 = sb2("m01", [128, 2]); kg0 = sb2("kg0b", [128, 1]); b2k = sb2("b2kb", [128, 1]); b2v = sb2("b2v2", [1, 64]); b2vb = sb2("b2vb2", [1, 64], BF16)
            onesb = sb2("onesb2", [1, 128], BF16); blk = sb2("blk2", [128, 128])
            hx = sb2("hx", [128, 256]); hg = sb2("hg", [128, 256]); hs = sb2("hs", [128, 256])
            hidb = sb2("hidb", [128, 2, 256], BF16)
            kcf = sb2("kcf", [128, 256]); kcs = sb2("kcs", [128, 256]); kr = sb2("kr", [128, 256]); kr2 = sb2("kr2", [128, 256])
            pt2 = ps2("pt2", [128, 4, 128], BF16)
            pb = ps2("pb", [128, 4])
            ph_ = [ps2("ph%d" % i, [128, 256]) for i in range(2)]
            pv = ps2("pv", [128, 2, 64])
            dma(kg0[:], d_kg0, [], ["kg0"]); dma(b2k[:], d_b2k, [], ["b2k"]); dma(b2v[:], d_b2v, [], ["b2v"])
            dma(m01[:], d_m01, [], ["kg0"])
            dma(blk[:], d_blk, [], ["blk"]); dma(peT[:], d_peT, [], ["peT"]); dma(b1T[:], d_b1T, [], ["b1T"])
            cp(b2vb[:], b2v[:], ["b2v"], ["b2vb"]); ms(onesb[:], 1.0, ["onesb"]); cp(peTb[:], peT[:], ["peT"], ["peTb"])
            dma(w2f[:], d_w2.rearrange("j (hc p) d -> p j hc d", p=128), [], ["w2f"])
            for dup in range(2):
                pass
            w2kd_v = w2kd
            for dup in range(2):
                cp(w2kd[:, :, dup, :], w2f[:, 0, :, :], ["w2f"], ["w2kd"])
            cp(w2v[:], w2f[:, 1, :, :], ["w2f"], ["w2v"])
            for j in range(2):
                dma(w1st[:], d_w1[j].rearrange("(kk p) h -> p kk h", p=128), [], ["w1st"])
                cp(w1b[:, j, :, :], w1st[:], ["w1st"], ["w1b"], eng=("pool" if j else "dve"))
            projp = proj.rearrange("(m l) n -> m l n", l=2)
            for mt in range(16):
                sl = mt % 2
                dma(pc[sl][:], projp[mt * 128:(mt + 1) * 128, :, 1024:1280], [], ["pc%d" % sl])
                cp(pcb[sl][:].rearrange("m c l d -> m l c d"), pc[sl][:].rearrange("m l (c d) -> m l c d", d=64),
                   ["pc%d" % sl], ["pcb%d" % sl], eng="pool")
                for cb in range(4):
                    tr(pt2[:, cb, :], pcb[sl][:, cb, :, :].rearrange("m l d -> m (l d)"), C.identB[:],
                       ["pcb%d" % sl, "identB"], ["pt2"])
                cp(PS[:, :, mt * 128:(mt + 1) * 128], pt2[:], ["pt2"], ["PS"], eng="act")
            for j in range(2):
                for hc in range(2):
                    for kk in range(16):
                        mm(pb[:, 2 * j + hc:2 * j + hc + 1], w1b[:, j, kk, hc * 128:(hc + 1) * 128], peTb[:, j, kk:kk + 1],
                           kk == 0, kk == 15, ["w1b", "peTb"], ["pb"])
            tt(bias1[:].rearrange("p a b -> p (a b)"), pb[:], b1T[:].rearrange("p a b -> p (a b)"), A, ["pb", "b1T"], ["bias1"])
            ms(hidb[:, :, 255:256], 0.0, ["hidb"])
            ms(vcA[:].rearrange("p a b c -> p (a b c)"), 0.0, ["vcA"])
            for j in range(2):
                for hk in range(2):
                    cb = 2 * j + hk
                    for hc in range(2):
                        for kk in range(16):
                            mm(ph_[hc][:, 0:255], w1b[:, j, kk, hc * 128:(hc + 1) * 128], PS[:, cb, kk:kk + 8 * 254 + 1:8],
                               kk == 0, kk == 15, ["w1b", "PS"], ["ph%d" % hc])
                        act(hx[:, 0:255], ph_[hc][:, 0:255], AF.Identity, ["ph%d" % hc, "bias1"], ["hx"], bias=bias1[:, j, hc:hc + 1])
                        tt(hg[:, 0:255], hx[:, 0:255], hx[:, 0:255], M, ["hx"], ["hg"])
                        ts(hg[:, 0:255], hg[:, 0:255], 0.044715, 1.0, M, A, ["hg"], ["hg"])
                        tt(hg[:, 0:255], hg[:, 0:255], hx[:, 0:255], M, ["hg", "hx"], ["hg"])
                        act(hs[:, 0:255], hg[:, 0:255], AF.Sigmoid, ["hg"], ["hs"], scale=KG)
                        tt(hidb[:, hc, 0:255], hs[:, 0:255], hx[:, 0:255], M, ["hs", "hx"], ["hidb"])
                    if j == 0:
                        for hc in range(2):
                            mm(ph_[0][:], w2kd[:, hc, :, :].rearrange("p a d -> p (a d)"), hidb[:, hc, :], hc == 0, hc == 1,
                               ["w2kd", "hidb"], ["ph0"])
                        act(kcf[:], ph_[0][:], AF.Identity, ["ph0", "b2k"], ["kcf"], bias=b2k[:])
                        tt(kcs[:], kcf[:], kcf[:], M, ["kcf"], ["kcs"])
                        mm(ph_[1][:], blk[:], kcs[:], True, True, ["blk", "kcs"], ["ph1"])
                        ts(kr[:], ph_[1][:], 1.0 / 64, RMS_EPS, M, A, ["ph1"], ["kr"])
                        act(kr2[:], kr[:], AF.Sqrt, ["kr"], ["kr2"])
                        sc.op("dve", lambda e: e.reciprocal(out=kr[:], in_=kr2[:]), ["kr2"], ["kr"])
                        tt(kcf[:], kcf[:], kr[:], M, ["kcf", "kr"], ["kcf"])
                        for par in range(2):
                            ts(kcT2[:, hk, par, :], kcf[:], kg0[:, 0:1], m01[:, par:par + 1], M, M, ["kcf", "kg0"], ["kcT2"])
                    else:
                        for ct in range(2):
                            for hc in range(2):
                                mm(pv[:, ct, :], hidb[:, hc, ct * 128:(ct + 1) * 128], w2v[:, hc, :], hc == 0, False,
                                   ["hidb", "w2v"], ["pv"])
                            mm(pv[:, ct, :], onesb[0:1, :], b2vb[0:1, :], False, True, ["onesb", "b2vb"], ["pv"])
                        cp(vcA[:, hk, :, 0:64], pv[:], ["pv"], ["vcA"])
                        ms(vcA[:, hk, :, 64:65], 1.0, ["vcA"])
            sc.barrier()
        if NSTOP < 3:
            return
        with ExitStack() as p3:
            sb3 = lambda name, shape, dt=F32: p3.enter_context(nc.sbuf_tensor(uniq(name), list(shape), dt))
            ps3 = lambda name, shape, dt=F32: p3.enter_context(nc.psum_tensor(uniq(name), list(shape), dt))
            auxl = sb3("auxl", [128, 128], BF16); auxr = sb3("auxr", [128, 8, 512], BF16)
            biasS = sb3("biasS", [128, 8, 35]); biasC = sb3("biasC", [128, 8, 8, 2])
            negm = sb3("negm", [128, 19, 512], BF16); mvadd = sb3("mvadd", [128, 32, 2, 64], BF16)
            ebc = sb3("ebc", [128, 32, 128], BF16)
            tmp = [sb3("tmp%d" % i, [128, 512]) for i in range(4)]
            pex = [sb3("pex%d" % i, [128, 512], BF16) for i in range(4)]
            facw = sb3("facw", [128, 4]); facw2 = sb3("facw2", [128, 4]); otmpw = sb3("otmpw", [128, 4, 64])
            nselb = sb3("nselb", [128, 4, 2, 64], BF16)
            oacc = sb3("oacc", [128, 4, 8, 64])
            oaccb = sb3("oaccb", [128, 4, 512], BF16)
            otmp = sb3("otmp", [128, 4, 64])
            impacc = sb3("impacc", [128, 4, 64]); imp2 = sb3("imp2", [128, 4, 64]); selm = sb3("selm", [128, 4, 64])
            top8 = sb3("top8", [128, 4, 8])
            selT = sb3("selT", [128, 512], BF16)
            nst = sb3("nst", [128, 4, 4, 128], BF16)
            fac = sb3("fac", [128, 4]); fac2 = sb3("fac2", [128, 4])
            psS = [ps3("psS%d" % i, [128, 512]) for i in range(3)]
            psOsL = [ps3("psOs%d" % i, [128, 4, 65]) for i in range(2)]
            psOwL = [ps3("psOw%d" % i, [128, 4, 65]) for i in range(2)]
            ptr3 = ps3("ptr3", [128, 4, 128], BF16)
            dma(auxl[:], d_auxl, [], ["aux"]); dma(auxr[:], d_auxr, [], ["aux"])
            dma(biasS[:], d_biasS, [], ["bias"]); dma(biasC[:], d_biasC, [], ["bias"])
            dma(negm[:], d_negm, [], ["negm"]); dma(mvadd[:], d_mvadd, [], ["mvadd"]); dma(ebc[:], d_ebc, [], ["ebc"])
            cnt = [0]
            NS = 3
            LOOK = 2
            from collections import deque
            pend = deque()

            def submit(front, back):
                if front is not None:
                    front()
                pend.append(back)
                while len(pend) > LOOK:
                    pend.popleft()()

            def flush():
                while pend:
                    pend.popleft()()

            def mm_skip(o, l, rr, st, sp, r, w):
                sc.op("pe", lambda e: e.matmul(o, lhsT=l, rhs=rr, start=st, stop=sp, skip_group_check=True), r, w)

            def chunk_task(kT, kname, hk, h, kcol, Q, bias_ap, nm_idx, use_sel, pvs):
                i = cnt[0] % NS
                cnt[0] += 1
                pb_ = 64 * (h % 2)
                PSN, TMP, PEX = "psS%d" % i, "tmp%d" % i, "pex%d" % i

                def front():
                    mm(psS[i][:], kT[:, hk, h % 2, kcol * 128:(kcol + 1) * 128],
                       qT[:, h // 2, Q * 512:(Q + 1) * 512], True, False, [kname, "qT"], [PSN])
                    mm(psS[i][:], auxl[:, :], auxr[:, h, :], False, not use_sel, ["aux"], [PSN])
                    if use_sel:
                        mm(psS[i][:], ebc[:, kcol, :], selT[:, :], False, True, ["ebc", "selT"], [PSN])

                def back():
                    if nm_idx is not None:
                        tt(tmp[i][:], psS[i][:], negm[:, nm_idx, :], A, [PSN, "negm"], [TMP])
                        act(pex[i][:], tmp[i][:], AF.Exp, [TMP, "bias"], [PEX], bias=bias_ap)
                    else:
                        act(pex[i][:], psS[i][:], AF.Exp, [PSN, "bias"], [PEX], bias=bias_ap)
                    for (view, rhs, rname, pname, first, last) in pvs:
                        for sub in range(4):
                            mm_skip(view(sub), pex[i][:, sub * 128:(sub + 1) * 128], rhs, first and sub == 0, last,
                                    [PEX, rname], [pname])
                submit(front, back)

            def post_cmp(Q, h, g_):
                psOs, psOw = psOsL[g_ % 2], psOwL[g_ % 2]
                PSn, PWn = "psOs%d" % (g_ % 2), "psOw%d" % (g_ % 2)
                psI = psOw[:, :, 0:64]

                def back():
                    ts(fac[:], psOs[:, :, 64], 1e-20, None, ALU.max, None, [PSn], ["fac"])
                    sc.op("dve", lambda e: e.reciprocal(out=fac2[:], in_=fac[:]), ["fac"], ["fac2"])
                    bc4 = fac2[:].unsqueeze(2).to_broadcast([128, 4, 64])
                    if g_ == 0:
                        tt(impacc[:], psI, bc4, M, [PWn, "fac2"], ["impacc"])
                    else:
                        tt(imp2[:], psI, bc4, M, [PWn, "fac2"], ["imp2"])
                        tt(impacc[:], impacc[:], imp2[:], A, ["imp2", "impacc"], ["impacc"])
                    tt(fac[:], fac2[:], gn[:, 4 * Q:4 * Q + 4, h], M, ["fac2", "gn"], ["fac"])
                    tt(oacc[:, :, h, :], psOs[:, :, 0:64], fac[:].unsqueeze(2).to_broadcast([128, 4, 64]), M,
                       [PSn, "fac"], ["oacc"])
                submit(None, back)

            def post_branch(Q, h, br):
                psO, nm_ = (psOsL[h % 2], "psOs%d" % (h % 2)) if br == 1 else (psOwL[h % 2], "psOw%d" % (h % 2))
                fa, fb, ot_ = (fac, fac2, otmp) if br == 1 else (facw, facw2, otmpw)
                fan, fbn, otn = ("fac", "fac2", "otmp") if br == 1 else ("facw", "facw2", "otmpw")

                def back():
                    ts(fa[:], psO[:, :, 64], 1e-20, None, ALU.max, None, [nm_], [fan])
                    sc.op("dve", lambda e: e.reciprocal(out=fb[:], in_=fa[:]), [fan], [fbn])
                    tt(fa[:], fb[:], gn[:, 4 * Q:4 * Q + 4, 8 * br + h], M, [fbn, "gn"], [fan])
                    tt(ot_[:], psO[:, :, 0:64], fa[:].unsqueeze(2).to_broadcast([128, 4, 64]), M, [nm_, fan], [otn])
                    tt(oacc[:, :, h, :], oacc[:, :, h, :], ot_[:], A, [otn, "oacc"], ["oacc"], eng="pool")
                submit(None, back)

            for Q in range(8):
                for hk in range(2):
                    for g_ in range(4):
                        h = 4 * hk + g_
                        cts = [ct for ct in range(2) if 512 * Q - 2048 * ct + 480 >= 0]
                        for ci, ct in enumerate(cts):
                            dl = 512 * Q - 2048 * ct
                            nm = None if dl >= 2063 else (dl // 512 if ct == 0 else 15 + dl // 512)
                            first, last = ci == 0, ci == len(cts) - 1
                            chunk_task(kcT2, "kcT2", hk, h, ct, Q, biasC[:, h, Q, ct:ct + 1], nm, False,
                                       [(lambda sub, g_=g_: psOsL[g_ % 2][:, sub, :], vcA[:, hk, ct, :], "vcA", "psOs%d" % (g_ % 2), first, last),
                                        (lambda sub, g_=g_: psOwL[g_ % 2][:, sub, 0:64], ovl[:, ct, :], "ovl", "psOw%d" % (g_ % 2), first, last)])
                        post_cmp(Q, h, g_)
                    flush()
                    tt(imp2[:], impacc[:], mvadd[:, 4 * Q:4 * Q + 4, 0, :], M, ["impacc", "mvadd"], ["imp2"])
                    tt(imp2[:], imp2[:], mvadd[:, 4 * Q:4 * Q + 4, 1, :], A, ["imp2", "mvadd"], ["imp2"])
                    for sub in range(4):
                        sc.op("dve", lambda e, sub=sub: e.max(out=top8[:, sub, :], in_=imp2[:, sub, :]), ["imp2"], ["top8"])
                    for sub in range(4):
                        ts(selm[:, sub, :], imp2[:, sub, :], top8[:, sub, 7:8], None, ALU.is_ge, None, ["imp2", "top8"], ["selm"])
                    for dup in range(2):
                        ts(nselb[:, :, dup, :], selm[:], -NEGV, NEGV, M, A, ["selm"], ["nselb"])
                    for sub in range(4):
                        tr(ptr3[:, sub, :], nselb[:, sub, :, :].rearrange("p a d -> p (a d)"), C.identB[:],
                           ["nselb", "identB"], ["ptr3"])
                    cp(selT[:], ptr3[:].rearrange("p a b -> p (a b)"), ["ptr3"], ["selT"], eng="act")
                    for g_ in range(4):
                        h = 4 * hk + g_
                        kws = [kc for kc in range(4 * Q - 2, 4 * Q + 4) if kc >= 0]
                        for ci, kc in enumerate(kws):
                            dl = 4 * Q - kc
                            chunk_task(kwT2, "kwT2", hk, h, kc, Q, biasS[:, h, dl + 3:dl + 4], 9 + (2 - dl), False,
                                       [(lambda sub, h=h: psOwL[h % 2][:, sub, :], vw_aug[:, kc, hk, :], "vw_aug", "psOw%d" % (h % 2), ci == 0, ci == len(kws) - 1)])
                        post_branch(Q, h, 2)
                    for g_ in range(4):
                        h = 4 * hk + g_
                        sl_ = SLOPES[h]
                        kcs_ = [kc for kc in range(4 * Q + 4) if not (sl_ * (128 * (4 * Q - kc) - 127) > 115.0)]
                        for ci, kc in enumerate(kcs_):
                            dl = 4 * Q - kc
                            chunk_task(ksT2, "ksT2", hk, h, kc, Q, biasS[:, h, dl + 3:dl + 4], (5 - dl) if dl <= 0 else None, True,
                                       [(lambda sub, h=h: psOsL[h % 2][:, sub, :], vs_aug[:, kc, hk, :], "vs_aug", "psOs%d" % (h % 2), ci == 0, ci == len(kcs_) - 1)])
                        post_branch(Q, h, 1)
                    flush()
                cp(oaccb[:], oacc[:].rearrange("p a h d -> p a (h d)"), ["oacc"], ["oaccb"])
                for sub in range(4):
                    for c4 in range(4):
                        tr(ptr3[:, c4, :], oaccb[:, sub, c4 * 128:(c4 + 1) * 128], C.identB[:], ["oaccb", "identB"], ["ptr3"])
                    cp(nst[:, sub, :, :], ptr3[:], ["ptr3"], ["nst"], eng="act")
                dma(C.nsaT_d[:, Q * 4:(Q + 1) * 4, :, :], nst[:], ["nst"], ["nsaT_d"])
            sc.barrier()


def phase_e(C):
    nc, sc = C.nc, C.sc
    H = mk_helpers(sc)
    tt, ts, stt, act, cp, ms, mm, tr, dma = H.tt, H.ts, H.stt, H.act, H.cp, H.ms, H.mm, H.tr, H.dma
    M, A, SUB = ALU.mult, ALU.add, ALU.subtract
    din = C.din
    d_wa = din("w_branch_a", [512, D]); d_wb = din("w_branch_b", [512, D]); d_wo = din("w_out", [D, D])
    d_g2 = din("g_ffn", [1, D]); d_wr = din("w_router", [D, 64]); d_rb = din("router_bias", [1, 64])
    with ExitStack() as ph:
        sbp = lambda name, shape, dt=F32: ph.enter_context(nc.sbuf_tensor(uniq(name), list(shape), dt))
        psp = lambda name, shape, dt=F32: ph.enter_context(nc.psum_tensor(uniq(name), list(shape), dt))
        modE = sbp("modE", [128, 3 * D])
        dma(modE[:], C.mod_d[:, 2 * D:5 * D], ["mod_d"], ["modB"])
        wa = sbp("wa", [128, 4, D], BF16); wb = sbp("wb", [128, 4, D], BF16); wo = sbp("wo", [128, 8, D], BF16)
        wr = sbp("wr", [128, 8, 64]); rb = sbp("rb", [128, 64]); G2 = sbp("G2", [128, D])
        wrh = sbp("wrh", [128, 8, 64], BF16); wrl = sbp("wrl", [128, 8, 64], BF16)
        with ExitStack() as pw:
            wst = pw.enter_context(nc.sbuf_tensor(uniq("west"), [128, 8, D], F32))
            dma(wst[:, 0:4, :], d_wa.rearrange("(kc p) n -> p kc n", p=128), [], ["west"])
            cp(wa[:], wst[:, 0:4, :], ["west"], ["wa"], eng="pool")
            dma(wst[:, 4:8, :], d_wb.rearrange("(kc p) n -> p kc n", p=128), [], ["west2"])
            cp(wb[:], wst[:, 4:8, :], ["west2"], ["wb"])
            dma(wst[:], d_wo.rearrange("(kc p) n -> p kc n", p=128), ["west2"], ["west", "west2"])
            cp(wo[:, 0:4, :], wst[:, 0:4, :], ["west", "west2"], ["wo"], eng="pool")
            cp(wo[:, 4:8, :], wst[:, 4:8, :], ["west", "west2"], ["wo"])
            dma(wr[:], d_wr.rearrange("(kc p) n -> p kc n", p=128), [], ["wr"])
            dma(rb[:], d_rb.partition_broadcast(128), [], ["rb"])
            cp(wrh[:], wr[:], ["wr"], ["wrh"])
            tt(wrl[:], wr[:], wrh[:], SUB, ["wr", "wrh"], ["wrh"])
            dma(G2[:], d_g2.partition_broadcast(128), [], ["G2"])
            stt(G2[:], modE[:, 2 * D:3 * D], 1.0, G2[:], A, M, ["G2", "modB"], ["G2"])
            sc.barrier()
        NB = 3
        mk = lambda nm, shape, dt=F32, n=NB: [sbp("%s%d" % (nm, i), shape, dt) for i in range(n)]
        s5t = mk("s5t", [128, 4, 128], BF16); nst = mk("nsat", [128, 4, 128], BF16); gm = mk("gm", [128, 2048], BF16)
        xt = mk("xe", [128, D]); mb = mk("mb", [128, D], BF16); mT = mk("mT", [128, 8, 128], BF16)
        x1t = mk("x1t", [128, D], n=4); h2 = mk("h2", [128, D], n=3)
        h2hi = mk("h2hi", [128, D], BF16, n=4); h2lo = mk("h2lo", [128, D], BF16, n=3)
        h2Tb = mk("h2Tb", [128, 8, 128], BF16, n=8); h2Tl = mk("h2Tl", [128, 8, 128], BF16, n=8)
        m1 = sbp("m1", [128, 512]); m2 = sbp("m2", [128, 512]); m3 = sbp("m3", [128, 512])
        junk = sbp("junk2", [128, D], BF16); stat = mk("stat2", [128, 4], n=4); tmpn = sbp("tmpn2", [128, D])
        scs = sbp("scs", [128, 4, 64]); selv = sbp("selv", [128, 4, 64]); eq = sbp("eq", [128, 4, 64]); sel2 = sbp("sel2", [128, 4, 64])
        mx1 = sbp("mx1", [128, 4, 8]); mx2 = sbp("mx2", [128, 4, 8]); gs = sbp("gs", [128, 4, 8]); t8 = sbp("t8", [128, 4, 8])
        gmk = sbp("gmk", [128, 4, 8]); gng = sbp("gng", [128, 4, 8]); den = sbp("den", [128, 8])
        pA = psp("pA", [128, 512]); pB = psp("pB", [128, 512])
        pW = [psp("pW%d" % i, [128, 512]) for i in range(2)]
        pTm = psp("pTm", [128, 8, 128], BF16); pTh = psp("pTh", [128, 8, 128], BF16); pTl = psp("pTl", [128, 8, 128], BF16)
        pL = psp("pL", [128, 4, 64])
        n_ = lambda nm, t: "%s%d" % (nm, t % NB)

        def st0(t):
            sl = t % NB
            dma(s5t[sl][:], C.s5T_d[:, t, :, :], ["s5T_d"], [n_("s5t", t)])
            dma(nst[sl][:], C.nsaT_d[:, t, :, :], ["nsaT_d"], [n_("nsat", t)])
            dma(gm[sl][:], C.gms[t * 128:(t + 1) * 128, :], ["gms"], [n_("gm", t)])
            dma(xt[sl][:], C.x_in[t * 128:(t + 1) * 128, :], [], [n_("xe", t)])

        def st1(t):
            sl = t % NB
            for half in range(2):
                for kc in range(4):
                    mm(pA[:], s5t[sl][:, kc, :], wa[:, kc, half * 512:(half + 1) * 512], kc == 0, kc == 3, [n_("s5t", t), "wa"], ["pA"])
                for kc in range(4):
                    mm(pB[:], nst[sl][:, kc, :], wb[:, kc, half * 512:(half + 1) * 512], kc == 0, kc == 3, [n_("nsat", t), "wb"], ["pB"])
                tt(m1[:], pA[:], gm[sl][:, half * 512:(half + 1) * 512], M, ["pA", n_("gm", t)], ["m1"])
                tt(m2[:], pB[:], gm[sl][:, 1024 + half * 512:1024 + (half + 1) * 512], M, ["pB", n_("gm", t)], ["m2"])
                tt(mb[sl][:, half * 512:(half + 1) * 512], m1[:], m2[:], A, ["m1", "m2"], [n_("mb", t)], eng="pool")

        def st2(t):
            sl = t % NB
            for kc in range(8):
                tr(pTm[:, kc, :], mb[sl][:, kc * 128:(kc + 1) * 128], C.identB[:], [n_("mb", t), "identB"], ["pTm"])
            cp(mT[sl][:], pTm[:], ["pTm"], [n_("mT", t)], eng="act")

        def st3(t):
            sl = t % NB
            s4 = t % 4
            X1, ST = "x1t%d" % s4, "stat%d" % s4
            for half in range(2):
                for kc in range(8):
                    mm(pW[half][:], mT[sl][:, kc, :], wo[:, kc, half * 512:(half + 1) * 512], kc == 0, kc == 7, [n_("mT", t), "wo"], ["pW%d" % half])
                tt(m3[:], pW[half][:], modE[:, half * 512:(half + 1) * 512], M, ["pW%d" % half, "modB"], ["m3"])
                tt(x1t[s4][:, half * 512:(half + 1) * 512], m3[:], xt[sl][:, half * 512:(half + 1) * 512], A, ["m3", n_("xe", t)], [X1])
            dma(C.x1[t * 128:(t + 1) * 128, :], x1t[s4][:], [X1], ["x1"])
            act(junk[:], x1t[s4][:], AF.Square, [X1], ["junk", ST], accum_out=stat[s4][:, 0:1])

        def st3b(t):
            s4 = t % 4
            ST = "stat%d" % s4
            ts(stat[s4][:, 1:2], stat[s4][:, 0:1], 1.0 / D, RMS_EPS, M, A, [ST], [ST])
            act(stat[s4][:, 2:3], stat[s4][:, 1:2], AF.Sqrt, [ST], [ST])

        def st3c(t):
            s4 = t % 4
            X1, ST, H2 = "x1t%d" % s4, "stat%d" % s4, "h2_%d" % (t % 3)
            sc.op("dve", lambda e: e.reciprocal(out=stat[s4][:, 3:4], in_=stat[s4][:, 2:3]), [ST], [ST])
            stt(tmpn[:], x1t[s4][:], stat[s4][:, 3:4], G2[:], M, M, [X1, ST, "G2"], ["tmpn"])
            tt(h2[t % 3][:], tmpn[:], modE[:, D:2 * D], A, ["tmpn", "modB"], [H2])
            cp(h2hi[s4][:], h2[t % 3][:], [H2], ["h2hi%d" % s4], eng="pool")

        def st3d(t):
            s4 = t % 4
            H2 = "h2_%d" % (t % 3)
            tt(h2lo[t % 3][:], h2[t % 3][:], h2hi[s4][:], SUB, [H2, "h2hi%d" % s4], ["h2lo%d" % (t % 3)])

        def st4(t):
            s4, s3, s8 = t % 4, t % 3, t % 8
            for kc in range(8):
                tr(pTh[:, kc, :], h2hi[s4][:, kc * 128:(kc + 1) * 128], C.identB[:], ["h2hi%d" % s4, "identB"], ["pTh"])
            cp(h2Tb[s8][:], pTh[:], ["pTh"], ["h2Tb%d" % s8], eng="act")
            for kc in range(8):
                tr(pTl[:, kc, :], h2lo[s3][:, kc * 128:(kc + 1) * 128], C.identB[:], ["h2lo%d" % s3, "identB"], ["pTl"])
            cp(h2Tl[s8][:], pTl[:], ["pTl"], ["h2Tl%d" % s8])
            dma(C.h2T_d[:, t, :, :], h2Tb[s8][:], ["h2Tb%d" % s8], ["h2T_d"])

        def st5(t):
            if t % 4 != 3:
                return
            t0 = t - 3
            for i in range(4):
                s8 = (t0 + i) % 8
                k_ = 0
                for kc in range(8):
                    for a_, an_, b_ in ((h2Tb[s8], "h2Tb%d" % s8, wrh), (h2Tb[s8], "h2Tb%d" % s8, wrl), (h2Tl[s8], "h2Tl%d" % s8, wrh)):
                        mm(pL[:, i, :], a_[:, kc, :], b_[:, kc, :], k_ == 0, k_ == 23, [an_, "wrh"], ["pL"])
                        k_ += 1
            act(scs[:], pL[:], AF.Sigmoid, ["pL"], ["scs"])
            R = ["rt"]
            f2 = lambda ap: ap.rearrange("p a b -> p (a b)")
            v3 = lambda ap: ap.rearrange("p a (g k) -> p (a g) k", k=8)
            b8 = lambda ap: f2(ap).unsqueeze(2).to_broadcast([128, 32, 8])
            tt(selv[:], scs[:], rb[:].unsqueeze(1).to_broadcast([128, 4, 64]), A, ["scs", "rb"], R)
            sc.op("dve", lambda e: e.tensor_reduce(out=f2(mx1[:]), in_=v3(selv[:]), axis=AX.X, op=ALU.max), R, R)
            tt(v3(eq[:]), v3(selv[:]), b8(mx1[:]), ALU.is_equal, R, R)
            stt(f2(sel2[:]), f2(eq[:]), -1e9, f2(selv[:]), M, A, R, R)
            sc.op("dve", lambda e: e.tensor_reduce(out=f2(mx2[:]), in_=v3(sel2[:]), axis=AX.X, op=ALU.max), R, R)
            tt(gs[:], mx1[:], mx2[:], A, R, R)
            for i in range(4):
                sc.op("dve", lambda e, i=i: e.max(out=t8[:, i, :], in_=gs[:, i, :]), R, ["t8_%d" % i])
            tt(gmk[:], gs[:], t8[:, :, 3:4].to_broadcast([128, 4, 8]), ALU.is_ge, R + ["t8_%d" % i for i in range(4)], R)
            ts(f2(gng[:]), f2(gmk[:]), 1e9, -1e9, M, A, R, R)
            tt(v3(sel2[:]), v3(selv[:]), b8(gmk[:]), M, R, R)
            tt(v3(sel2[:]), v3(sel2[:]), b8(gng[:]), A, R, R)
            for i in range(4):
                sc.op("dve", lambda e, i=i: e.max(out=t8[:, i, :], in_=sel2[:, i, :]), R, ["t8_%d" % i])
            tt(eq[:], sel2[:], t8[:, :, 7:8].to_broadcast([128, 4, 64]), ALU.is_ge, R + ["t8_%d" % i for i in range(4)], R)
            tt(f2(eq[:]), f2(eq[:]), f2(scs[:]), M, R + ["scs"], R)
            sc.op("dve", lambda e: e.tensor_reduce(out=den[:, 0:4], in_=eq[:], axis=AX.X, op=ALU.add), R, R)
            sc.op("dve", lambda e: e.reciprocal(out=den[:, 4:8], in_=den[:, 0:4]), R, R)
            stt(C.gates[:, t0:t0 + 4, :], eq[:], 2.5, den[:, 4:8].unsqueeze(2).to_broadcast([128, 4, 64]), M, M, R, ["gates"])

        stages = [st0, st1, st2, st3, st3b, st3c, st3d, st4, st5]
        for step in range(NT + len(stages) - 1):
            for k in reversed(range(len(stages))):
                t = step - k
                if 0 <= t < NT:
                    stages[k](t)
        if C.dbg:
            gd = C.dscr("gates_d", [128, 32, 64])
            dma(gd, C.gates[:], ["gates"], ["gates_d"])
        sc.barrier()


def phase_moe(C):
    nc, sc = C.nc, C.sc
    H = mk_helpers(sc)
    tt, ts, stt, act, cp, ms, mm, tr, dma = H.tt, H.ts, H.stt, H.act, H.cp, H.ms, H.mm, H.tr, H.dma
    M, A = ALU.mult, ALU.add
    din = C.din
    d_wg = din("w_gate", [64, D, 256]); d_wu = din("w_up", [64, D, 256]); d_wd = din("w_down", [64, 256, D])
    d_sg = din("ws_gate", [D, 256]); d_su = din("ws_up", [D, 256]); d_sd = din("ws_down", [256, D])
    NE = int(C.n_experts)
    with ExitStack() as ph:
        sbp = lambda name, shape, dt=F32: ph.enter_context(nc.sbuf_tensor(uniq(name), list(shape), dt))
        psp = lambda name, shape, dt=F32: ph.enter_context(nc.psum_tensor(uniq(name), list(shape), dt))
        h2T = sbp("h2T", [128, 16, 8, 128], BF16)
        gf = sbp("gf", [128, D])
        dma(gf[:], C.mod_d[:, 5 * D:6 * D], ["mod_d"], ["modB"])
        acc = sbp("acc", [128, 16, D])
        wgs = sbp("wgs", [128, 8, 256]); wus = sbp("wus", [128, 8, 256]); wds = sbp("wds", [128, 2, D])
        wgb = [sbp("wgb%d" % i, [128, 8, 256], BF16) for i in range(2)]
        wub = [sbp("wub%d" % i, [128, 8, 256], BF16) for i in range(2)]
        wdb = [sbp("wdb%d" % i, [128, 2, D], BF16) for i in range(2)]
        sg = [sbp("sg%d" % i, [128, 512]) for i in range(2)]
        hid = [sbp("hid%d" % i, [128, 2, 512], BF16) for i in range(2)]
        x1t = [sbp("x1m%d" % i, [128, D]) for i in range(2)]
        ot = [sbp("ot%d" % i, [128, D]) for i in range(2)]
        pG = [psp("pG%d" % i, [128, 512]) for i in range(2)]
        pU = [psp("pU%d" % i, [128, 512]) for i in range(2)]
        pD = [psp("pD%d" % i, [128, 512]) for i in range(4)]
        dcount = [0]
        pend_back = [None]
        for half in range(2):
            dma(h2T[:], C.h2T_d[:, half * 16:(half + 1) * 16, :, :], ["h2T_d"], ["h2T"])
            ms(acc[:].rearrange("p a b -> p (a b)"), 0.0, ["acc"], eng="pool")
            for e in range(-1, NE):
                sl = (e + 1) % 2
                if e < 0:
                    srcs = (d_sg, d_su, d_sd)
                else:
                    srcs = (d_wg[e], d_wu[e], d_wd[e])
                dma(wgs[:], srcs[0].rearrange("(kc p) h -> p kc h", p=128), [], ["wgs"])
                dma(wus[:], srcs[1].rearrange("(kc p) h -> p kc h", p=128), [], ["wus"])
                dma(wds[:], srcs[2].rearrange("(hc p) n -> p hc n", p=128), [], ["wds"])
                cp(wgb[sl][:], wgs[:], ["wgs"], ["wgb%d" % sl], eng="pool")
                cp(wub[sl][:], wus[:], ["wus"], ["wub%d" % sl], eng="pool")
                cp(wdb[sl][:], wds[:], ["wds"], ["wdb%d" % sl], eng="pool")
                for tg in range(4):
                    def front_q(q, tg=tg, sl=sl):
                        hs_ = tg % 2
                        hc = q // 2
                        if q % 2 == 0:
                            for kc in range(8):
                                mm(pG[hc][:], wgb[sl][:, kc, hc * 128:(hc + 1) * 128], h2T[:, tg * 4:(tg + 1) * 4, kc, :],
                                   kc == 0, kc == 7, ["wgb%d" % sl, "h2T"], ["pG%d" % hc])
                            act(sg[hc][:], pG[hc][:], AF.Silu, ["pG%d" % hc], ["sg%d" % hc])
                        else:
                            for kc in range(8):
                                mm(pU[hc][:], wub[sl][:, kc, hc * 128:(hc + 1) * 128], h2T[:, tg * 4:(tg + 1) * 4, kc, :],
                                   kc == 0, kc == 7, ["wub%d" % sl, "h2T"], ["pU%d" % hc])
                            tt(hid[hs_][:, hc, :], sg[hc][:], pU[hc][:], M, ["sg%d" % hc, "pU%d" % hc], ["hid%d" % hs_])

                    def back_q(q, tg=tg, sl=sl, e=e):
                        hs_ = tg % 2
                        sub = q
                        tile_ = tg * 4 + sub
                        for nh in range(2):
                            pd = dcount[0] % 4
                            dcount[0] += 1
                            for hc in range(2):
                                mm(pD[pd][:], hid[hs_][:, hc, sub * 128:(sub + 1) * 128], wdb[sl][:, hc, nh * 512:(nh + 1) * 512],
                                   hc == 0, hc == 1, ["hid%d" % hs_, "wdb%d" % sl], ["pD%d" % pd])
                            gsc = 1.0 if e < 0 else C.gates[:, half * 16 + tile_, e:e + 1]
                            an = "acc%d_%d" % (tile_, nh)
                            stt(acc[:, tile_, nh * 512:(nh + 1) * 512], pD[pd][:], gsc, acc[:, tile_, nh * 512:(nh + 1) * 512],
                                M, A, ["pD%d" % pd, "acc", "gates"], [an])
                    prev = pend_back[0]
                    for q in range(4):
                        front_q(q)
                        if prev is not None:
                            prev(q)
                    pend_back[0] = back_q
            for q in range(4):
                pend_back[0](q)
            pend_back[0] = None
            for tl_ in range(16):
                t = half * 16 + tl_
                sl = tl_ % 2
                dma(x1t[sl][:], C.x1[t * 128:(t + 1) * 128, :], ["x1"], ["x1m%d" % sl])
                rd = ["acc"] + ["acc%d_%d" % (tl_, nh) for nh in range(2)]
                tt(ot[sl][:], acc[:, tl_, :], gf[:], M, rd + ["modB"], ["ot%d" % sl])
                tt(ot[sl][:], ot[sl][:], x1t[sl][:], A, ["ot%d" % sl, "x1m%d" % sl], ["ot%d" % sl], eng="pool")
                dma(C.out[t * 128:(t + 1) * 128, :], ot[sl][:], ["ot%d" % sl], ["out"])
            sc.barrier()


def phase_ab(C):
    nc, sc = C.nc, C.sc
    H = mk_helpers(sc)
    tt, ts, stt, act, cp, ms, mm, tr, dma = H.tt, H.ts, H.stt, H.act, H.cp, H.ms, H.mm, H.tr, H.dma
    M, A = ALU.mult, ALU.add
    x, w_in, g_mix, proj = C.x_in, C.w_in, C.g_mix, C.proj
    with ExitStack() as pa:
        sba = lambda name, shape, dt=F32: pa.enter_context(nc.sbuf_tensor(uniq(name), list(shape), dt))
        psa = lambda name, shape, dt=F32: pa.enter_context(nc.psum_tensor(uniq(name), list(shape), dt))
        Gm = sba("Gm", [128, D])
        modB = sba("modA", [128, 2 * D])
        dma(modB[:], C.mod_d[:, 0:2 * D], ["mod_d"], ["modB"])
        winb = sba("winb", [128, 8, INW], BF16)
        nch = [(0, 512), (512, 512), (1024, 512), (1536, 280), (1816, 512), (2328, 512), (2840, 512), (3352, 512)]
        with ExitStack() as pw:
            wst = [pw.enter_context(nc.sbuf_tensor(uniq("wst%d" % i), [128, 8, 512], F32)) for i in range(2)]
            dma(Gm[:], g_mix.partition_broadcast(128), [], ["Gm"])
            stt(Gm[:], modB[:, D:2 * D], 1.0, Gm[:], A, M, ["modB", "Gm"], ["Gm"])
            wiv = w_in.rearrange("(kc p) n -> p kc n", p=128)
            for ci, (n0, nw) in enumerate(nch):
                sl = ci % 2
                dma(wst[sl][:, :, 0:nw], wiv[:, :, n0:n0 + nw], [], ["wst%d" % sl])
                cp(winb[:, 0:4, n0:n0 + nw], wst[sl][:, 0:4, 0:nw], ["wst%d" % sl], ["winb"], eng="pool")
                cp(winb[:, 4:8, n0:n0 + nw], wst[sl][:, 4:8, 0:nw], ["wst%d" % sl], ["winb"])
            sc.barrier()
        NB = 3
        mk = lambda nm, shape, dt=F32, n=NB: [sba("%s%d" % (nm, i), shape, dt) for i in range(n)]
        xt = mk("xt", [128, D]); stat = mk("stat", [128, 4]); hb = mk("hb", [128, D], BF16)
        hT = mk("hT", [128, 8, 128], BF16)
        ob = mk("ob", [128, 1816], n=2); obg = mk("obg", [128, 2048], BF16, n=2)
        junk = sba("junk", [128, D], BF16); tmp = sba("tmpn", [128, D])
        pT = psa("pT", [128, 8, 128], BF16)
        pp = [psa("pp%d" % i, [128, 512]) for i in range(6)]
        n_ = lambda nm, t: "%s%d" % (nm, t % NB)
        pcnt = [0]

        def a0(t):
            dma(xt[t % NB][:], x[t * 128:(t + 1) * 128, :], [], [n_("xt", t)])

        def a1(t):
            sl = t % NB
            X, ST = n_("xt", t), n_("stat", t)
            act(junk[:], xt[sl][:], AF.Square, [X], ["junk", ST], accum_out=stat[sl][:, 0:1])
            ts(stat[sl][:, 1:2], stat[sl][:, 0:1], 1.0 / D, RMS_EPS, M, A, [ST], [ST])
            act(stat[sl][:, 2:3], stat[sl][:, 1:2], AF.Sqrt, [ST], [ST])

        def a2(t):
            sl = t % NB
            X, ST = n_("xt", t), n_("stat", t)
            sc.op("dve", lambda e: e.reciprocal(out=stat[sl][:, 3:4], in_=stat[sl][:, 2:3]), [ST], [ST])
            stt(tmp[:], xt[sl][:], stat[sl][:, 3:4], Gm[:], M, M, [X, ST, "Gm"], ["tmpn"])
            tt(hb[sl][:], tmp[:], modB[:, 0:D], A, ["tmpn", "modB"], [n_("hb", t)])

        def a3(t):
            sl = t % NB
            for kc in range(8):
                tr(pT[:, kc, :], hb[sl][:, kc * 128:(kc + 1) * 128], C.identB[:], [n_("hb", t), "identB"], ["pT"])
            cp(hT[sl][:], pT[:], ["pT"], [n_("hT", t)], eng="act")

        def a4(t):
            sl = t % NB
            s2 = t % 2
            OB, OG = "ob%d" % s2, "obg%d" % s2
            for ci, (n0, nw) in enumerate(nch):
                pb = pcnt[0] % 6
                pcnt[0] += 1
                for kc in range(8):
                    mm(pp[pb][:, 0:nw], hT[sl][:, kc, :], winb[:, kc, n0:n0 + nw], kc == 0, kc == 7, [n_("hT", t), "winb"], ["pp%d" % pb])
                if n0 >= 1816:
                    act(obg[s2][:, n0 - 1816:n0 - 1816 + nw], pp[pb][:, 0:nw], AF.Sigmoid, ["pp%d" % pb], [OG])
                else:
                    cp(ob[s2][:, n0:n0 + nw], pp[pb][:, 0:nw], ["pp%d" % pb], [OB])
            dma(proj[t * 128:(t + 1) * 128, :], ob[s2][:], [OB], ["proj"])
            dma(C.gms[t * 128:(t + 1) * 128, :], obg[s2][:], [OG], ["gms"])

        stages = [a0, a1, a2, a3, a4]
        for step in range(NT + len(stages) - 1):
            for k in reversed(range(len(stages))):
                t = step - k
                if 0 <= t < NT:
                    stages[k](t)
        sc.barrier()


def build(dbg=False, phases=None):
    if phases is None:
        phases = ALL_PHASES
    nc = bass.Bass("TRN2", target_bir_lowering=False)
    sc = Sched()
    C = Ctx()
    global LAST_SC
    LAST_SC = sc
    C.nc, C.sc, C.dbg, C.phases = nc, sc, dbg, phases

    def din(name, shape, dt=F32):
        return nc.dram_tensor(name, list(shape), dt, kind="ExternalInput").ap()

    def dscr(name, shape, dt=F32, producer=None):
        kind = "ExternalOutput" if dbg else "Internal"
        if dbg and producer is not None and producer not in phases:
            kind = "ExternalInput"
        return nc.dram_tensor(name, list(shape), dt, kind=kind).ap()
    C.din, C.dscr = din, dscr

    x = din("x", [S, D])
    cT = din("cT", [128, 8])
    w_ada = din("w_ada", [D, 6 * D])
    b_ada = din("b_ada", [1, 6 * D])
    g_mix = din("g_mix", [1, D])
    w_in = din("w_in", [D, INW])
    identf = din("identf", [128, 128])
    out = nc.dram_tensor("out", [S, D], F32, kind="ExternalOutput").ap()
    proj = dscr("proj", [S, 1816], producer="ab")
    C.gms = dscr("gms", [S, 2048], BF16, producer="ab")
    C.proj = proj
    C.yscr = dscr("yscr", [S, 512], producer="s5")
    C.s5T_d = dscr("s5T_d", [128, 32, 4, 128], BF16, producer="s5")
    C.nsaT_d = dscr("nsaT_d", [128, 32, 4, 128], BF16, producer="nsa")
    C.x1 = dscr("x1", [S, D], producer="e")
    C.h2T_d = dscr("h2T_d", [128, 32, 8, 128], BF16, producer="e")
    C.x_in = x
    C.mod_d = dscr("mod_d", [128, 6 * D], producer="0")
    C.w_in, C.g_mix = w_in, g_mix
    C.out = out
    import os
    C.n_experts = 64

    es = ExitStack()
    with es:
        def sb(name, shape, dt=F32):
            return es.enter_context(nc.sbuf_tensor(uniq(name), list(shape), dt))

        def ps(name, shape, dt=F32):
            return es.enter_context(nc.psum_tensor(uniq(name), list(shape), dt))

        sems = {e: es.enter_context(nc.semaphore("s_" + e)) for e in ENGS}
        dsems = [es.enter_context(nc.semaphore("d%d" % i)) for i in range(N_DSEM)]

        identF = sb("identF", [128, 128])
        identB = sb("identB", [128, 128], BF16)
        ones = sb("ones", [128, 128])
        sc.dma(lambda e: e.dma_start(out=identF[:], in_=identf), writes=["identF"])
        sc.op("dve", lambda e: e.tensor_copy(out=identB[:], in_=identF[:]), ["identF"], ["identB"])
        sc.op("dve", lambda e: e.memset(ones[:], 1.0), [], ["ones"])

        C.identF, C.identB, C.ones = identF, identB, ones
        with ExitStack() as p0:
          if "0" in phases:
              def sb0(name, shape, dt=F32):
                  return p0.enter_context(nc.sbuf_tensor(uniq(name), list(shape), dt))
              modB = sb0("modB", [128, 6 * D])
              csb = sb0("csb", [128, 8])
              csl = sb0("csl", [128, 8])
              lhsc = sb0("lhsc", [128, 8, 128])
              bada = sb0("bada", [1, 6 * D])
              wbuf = [sb0("wada%d" % i, [128, 8, 512]) for i in range(2)]
              psm = [p0.enter_context(nc.psum_tensor(uniq("psm%d" % i), [128, 512], F32)) for i in range(2)]
              sc.dma(lambda e: e.dma_start(out=csb[:], in_=cT), writes=["csb"])
              sc.dma(lambda e: e.dma_start(out=bada[:], in_=b_ada), writes=["bada"])
              sc.op("act", lambda e: e.activation(out=csl[:], in_=csb[:], func=AF.Silu), ["csb"], ["csl"])
              for kc in range(8):
                  sc.op("dve", lambda e, kc=kc: e.tensor_scalar(
                      out=lhsc[:, kc, :], in0=ones[:], scalar1=csl[:, kc:kc + 1], scalar2=None,
                      op0=ALU.mult), ["csl", "ones"], ["lhsc"])
              wv = w_ada.rearrange("(kc p) n -> p kc n", p=128)
              for n in range(12):
                  sl = n % 2
                  sc.dma(lambda e, n=n, sl=sl: e.dma_start(out=wbuf[sl][:], in_=wv[:, :, n * 512:(n + 1) * 512]),
                         writes=["wada%d" % sl])
                  for kc in range(8):
                      sc.op("pe", lambda e, kc=kc, sl=sl: e.matmul(
                          psm[sl][:], lhsT=lhsc[:, kc, :], rhs=wbuf[sl][:, kc, :], start=(kc == 0), stop=False),
                          ["lhsc", "wada%d" % sl], ["psm%d" % sl])
                  sc.op("pe", lambda e, n=n, sl=sl: e.matmul(
                      psm[sl][:], lhsT=ones[0:1, :], rhs=bada[0:1, n * 512:(n + 1) * 512], start=False, stop=True),
                      ["ones", "bada"], ["psm%d" % sl])
                  sc.op("act", lambda e, n=n, sl=sl: e.activation(
                      out=modB[:, n * 512:(n + 1) * 512], in_=psm[sl][:], func=AF.Identity),
                      ["psm%d" % sl], ["modB"])
              sc.dma(lambda e: e.dma_start(out=C.mod_d, in_=modB[:]), reads=["modB"], writes=["mod_d"])
              sc.barrier()

        if "ab" in phases:
            phase_ab(C)
            sc.barrier()

        if "s5" in phases:
            phase_s5(C)
            sc.barrier()
        if "nsa" in phases:
            phase_nsa(C)
            sc.barrier()
        C.gates = sb("gates", [128, 32, 64])
        if "e" in phases:
            phase_e(C)
            sc.barrier()
        elif dbg and "moe" in phases:
            gin = din("gates_in", [128, 32, 64])
            sc.dma(lambda e: e.dma_start(out=C.gates[:], in_=gin), writes=["gates"])
            sc.barrier()
        if "moe" in phases:
            phase_moe(C)
            sc.barrier()

        sc.barrier()
        run = sc.emit(nc, sems, dsems)
        with nc.Block() as block:
            @block.tensor
            def _(e):
                run("pe", e)

            @block.scalar
            def _(e):
                run("act", e)

            @block.vector
            def _(e):
                run("dve", e)

            @block.gpsimd
            def _(e):
                run("pool", e)

            @block.sync
            def _(e):
                run("sp", e)
    return nc


def s5_inputs(inp):
    f = lambda a: np.ascontiguousarray(a, dtype=np.float32)
    pl = lambda a: f(a.reshape(16, 2, 64).transpose(1, 2, 0).reshape(128, 16))
    ldt = inp["s5_log_dt"][0].reshape(16, 2).T
    b4 = lambda a: f(a.reshape(16, 2, 64, 16).transpose(1, 2, 0, 3).reshape(128, 256))
    c4 = lambda a: f(a.reshape(16, 2, 16, 64).transpose(1, 3, 0, 2).reshape(128, 256))
    m = np.zeros((128, 4), np.float32)
    m[:64, 0] = 1.0; m[64:, 1] = 1.0; m[:64, 2] = -1.0; m[64:, 3] = -1.0
    return {
        "s5_lamr": pl(inp["s5_lambda_re"][0]), "s5_lami": pl(inp["s5_lambda_im"][0]),
        "s5_ldt": f(np.repeat(ldt[:, None, :], 64, axis=1).reshape(128, 16)),
        "s5_bre": b4(inp["s5_b_re"][0]), "s5_bim": b4(inp["s5_b_im"][0]),
        "s5_cre": c4(inp["s5_c_re"][0]), "s5_cim": c4(inp["s5_c_im"][0]),
        "s5_dcol": f(np.tile(inp["s5_d"][0].T, (8, 1))),
        "mask01": m,
        "s5_wglu": f(inp["s5_w_glu"][0]),
        "s5_bgl": f(inp["s5_b_glu"][0].reshape(4, 128).T),
    }


def make_inputs(inp, b):
    f = lambda a: np.ascontiguousarray(a, dtype=np.float32)
    return {
        "x": f(inp["x"][b]),
        "cT": f(inp["c"][b].reshape(8, 128).T),
        "w_ada": f(inp["w_ada"][0]),
        "b_ada": f(inp["b_ada"][0][None, :]),
        "g_mix": f(inp["norm_mix_gain"][0][None, :]),
        "w_in": f(inp["w_in"][0]),
        "identf": np.eye(128, dtype=np.float32),
        **s5_inputs(inp),
        **nsa_inputs(inp),
        "w_branch_a": f(inp["w_branch_a"][0]), "w_branch_b": f(inp["w_branch_b"][0]), "w_out": f(inp["w_out"][0]),
        "g_ffn": f(inp["norm_ffn_gain"][0][None, :]), "w_router": f(inp["w_router"][0]),
        "router_bias": f(inp["router_bias"][0][None, :]),
        "w_gate": f(inp["w_gate"][0]), "w_up": f(inp["w_up"][0]), "w_down": f(inp["w_down"][0]),
        "ws_gate": f(inp["ws_gate"][0]), "ws_up": f(inp["ws_up"][0]), "ws_down": f(inp["ws_down"][0]),
    }


def kernel(**inputs):
    nc = build(dbg=False)
    in_maps = [make_inputs(inputs, b) for b in range(8)]
    res = run_bass_kernel_spmd(nc, in_maps, core_ids=list(range(8)))
    return np.stack([r["out"] for r in res.results], axis=0).astype(np.float32)
```
